# Optimizing a Trainium2 kernel written in Bass

```python
import math
import jax, jax.numpy as jnp
from jax import lax
import numpy as np

D_MODEL = 1024
BATCH = 8
SEQ = 4096
DEPTH = 1

HEAD_DIM = 64
N_FOX_HEADS = 8
N_DIL_HEADS = 8
FOX_WIDTH = N_FOX_HEADS * HEAD_DIM
DIL_WIDTH = N_DIL_HEADS * HEAD_DIM
MIX_WIDTH = FOX_WIDTH + DIL_WIDTH
IN_WIDTH = 3 * FOX_WIDTH + N_FOX_HEADS + 3 * DIL_WIDTH
DILATION_PATTERNS = ((128, 1), (512, 4), (2048, 16))
Q_BLOCK = 128
RMS_EPS = 1e-6
NEG_INF = -1e30

PEER_HEADS = 8
PEER_NKEYS = 128
PEER_EXPERTS = PEER_NKEYS * PEER_NKEYS
PEER_TOPK = 16
PEER_QDIM = 256
PEER_HALF = PEER_QDIM // 2
PEER_CHUNK = 128

kernel_name = "hybrid_fox_dilated_peer_block"


def rms_norm(x, g):
    xf = x.astype(jnp.float32)
    y = xf * lax.rsqrt(jnp.mean(xf * xf, axis=-1, keepdims=True) + RMS_EPS)
    return (y * g.astype(jnp.float32)).astype(x.dtype)


def split_heads(a, n_heads):
    b, t, _ = a.shape
    return a.reshape(b, t, n_heads, HEAD_DIM).transpose(0, 2, 1, 3)


def merge_heads(a):
    b, h, t, d = a.shape
    return a.transpose(0, 2, 1, 3).reshape(b, t, h * d)


def alibi_slopes(n):
    return 2.0 ** (-8.0 * jnp.arange(1, n + 1, dtype=jnp.float32) / n)


def fox_attention(q, k, v, log_f):
    b, h, t, hd = q.shape
    c = jnp.cumsum(log_f, axis=-1)
    scale = 1.0 / math.sqrt(hd)
    kpos = jnp.arange(t)

    def block(i):
        t0 = i * Q_BLOCK
        qb = lax.dynamic_slice_in_dim(q, t0, Q_BLOCK, axis=2)
        cq = lax.dynamic_slice_in_dim(c, t0, Q_BLOCK, axis=2)
        s = jnp.einsum('bhqd,bhkd->bhqk', qb, k).astype(jnp.float32) * scale
        s = s + cq[..., :, None] - c[:, :, None, :]
        qpos = t0 + jnp.arange(Q_BLOCK)
        mask = kpos[None, :] <= qpos[:, None]
        s = jnp.where(mask, s, NEG_INF)
        p = jax.nn.softmax(s, axis=-1)
        return jnp.einsum('bhqk,bhkd->bhqd', p.astype(v.dtype), v)

    out = lax.map(block, jnp.arange(t // Q_BLOCK))
    return out.transpose(1, 2, 0, 3, 4).reshape(b, h, t, hd)


def dilated_window_branch(q, k, v, window, dilation, slopes):
    b, h, t, hd = q.shape
    n_steps = window // dilation
    sub_len = t // dilation
    nb = -(-sub_len // Q_BLOCK)
    pad = nb * Q_BLOCK - sub_len

    def to_sub(a):
        a = a.reshape(b, h, sub_len, dilation, hd).transpose(0, 1, 3, 2, 4)
        a = jnp.pad(a, ((0, 0), (0, 0), (0, 0), (0, pad), (0, 0)))
        return a.reshape(b, h, dilation, nb, Q_BLOCK, hd)

    qs, ks, vs = to_sub(q), to_sub(k), to_sub(v)

    def with_prev(a):
        prev = jnp.concatenate([jnp.zeros_like(a[:, :, :, :1]), a[:, :, :, :-1]], axis=3)
        return jnp.concatenate([prev, a], axis=4)

    kc, vc = with_prev(ks), with_prev(vs)
    s = jnp.einsum('bhrnqd,bhrnkd->bhrnqk', qs, kc).astype(jnp.float32) / math.sqrt(hd)

    qi = jnp.arange(Q_BLOCK)[:, None] + Q_BLOCK
    ki = jnp.arange(2 * Q_BLOCK)[None, :]
    steps = qi - ki
    band = (steps >= 0) & (steps <= n_steps)
    key_sub = jnp.arange(nb)[:, None, None] * Q_BLOCK - Q_BLOCK + ki[None]
    valid = band[None] & (key_sub >= 0)
    bias = -slopes[:, None, None, None, None] * (dilation * steps).astype(jnp.float32)[None, None, None]
    s = jnp.where(valid, s + bias, NEG_INF)
    lse = jax.nn.logsumexp(s, axis=-1)
    p = jnp.exp(s - lse[..., None])
    o = jnp.einsum('bhrnqk,bhrnkd->bhrnqd', p.astype(vc.dtype), vc)

    def from_sub(a, tail):
        a = a.reshape((b, h, dilation, nb * Q_BLOCK) + tail)[:, :, :, :sub_len]
        a = jnp.swapaxes(a, 2, 3)
        return a.reshape((b, h, t) + tail)

    return from_sub(o, (hd,)), from_sub(lse, ())


def dilated_attention(q, k, v):
    slopes = alibi_slopes(q.shape[1])
    outs, lses = [], []
    for window, dilation in DILATION_PATTERNS:
        o, l = dilated_window_branch(q, k, v, window, dilation, slopes)
        outs.append(o)
        lses.append(l)
    w = jax.nn.softmax(jnp.stack(lses, axis=0), axis=0)
    o = jnp.stack(outs, axis=0).astype(jnp.float32)
    return jnp.sum(w[..., None] * o, axis=0).astype(q.dtype)


def peer_ffn(xn, wq, subkeys, u_table, v_table):
    b, t, d = xn.shape
    xt = xn.reshape(-1, PEER_CHUNK, d)

    def chunk(xc):
        q = (xc @ wq).reshape(PEER_CHUNK, PEER_HEADS, 2, PEER_HALF)
        s = jnp.einsum('chpd,hpkd->chpk', q, subkeys).astype(jnp.float32)
        s1, i1 = lax.top_k(s[:, :, 0], PEER_TOPK)
        s2, i2 = lax.top_k(s[:, :, 1], PEER_TOPK)
        cand = (s1[..., :, None] + s2[..., None, :]).reshape(PEER_CHUNK, PEER_HEADS, PEER_TOPK * PEER_TOPK)
        cidx = (i1[..., :, None] * PEER_NKEYS + i2[..., None, :]).reshape(PEER_CHUNK, PEER_HEADS, PEER_TOPK * PEER_TOPK)
        cs, ci = lax.top_k(cand, PEER_TOPK)
        eidx = jnp.take_along_axis(cidx, ci, axis=-1)
        g = jax.nn.softmax(cs, axis=-1)
        ug = jnp.take(u_table, eidx, axis=0)
        vg = jnp.take(v_table, eidx, axis=0)
        hid = jnp.einsum('cd,chkd->chk', xc, ug).astype(jnp.float32)
        a = jax.nn.gelu(hid, approximate=False) * g
        return jnp.einsum('chk,chkd->cd', a.astype(vg.dtype), vg)

    out = lax.map(chunk, xt)
    return out.reshape(b, t, d)


def setup_inputs(seed: int = 0) -> dict:
    key = jax.random.key(seed)
    ks = jax.random.split(key, 14)
    f32 = jnp.float32
    sd = D_MODEL ** -0.5
    x = jax.random.normal(ks[0], (BATCH, SEQ, D_MODEL), f32)
    norm1_g = 1.0 + 0.05 * jax.random.normal(ks[1], (DEPTH, D_MODEL), f32)
    w_in = jnp.concatenate([
        sd * jax.random.normal(ks[2], (DEPTH, D_MODEL, 3 * FOX_WIDTH), f32),
        0.1 * sd * jax.random.normal(ks[3], (DEPTH, D_MODEL, N_FOX_HEADS), f32),
        sd * jax.random.normal(ks[4], (DEPTH, D_MODEL, 3 * DIL_WIDTH), f32),
    ], axis=-1)
    b_forget = 3.0 + 0.1 * jax.random.normal(ks[5], (DEPTH, N_FOX_HEADS), f32)
    w_out = MIX_WIDTH ** -0.5 * jax.random.normal(ks[6], (DEPTH, MIX_WIDTH, D_MODEL), f32)
    norm2_g = 1.0 + 0.05 * jax.random.normal(ks[7], (DEPTH, D_MODEL), f32)
    peer_wq = sd * jax.random.normal(ks[8], (DEPTH, D_MODEL, PEER_HEADS * PEER_QDIM), f32)
    peer_subkeys = PEER_HALF ** -0.5 * jax.random.normal(ks[9], (DEPTH, PEER_HEADS, 2, PEER_NKEYS, PEER_HALF), f32)
    peer_u = sd * jax.random.normal(ks[10], (DEPTH, PEER_EXPERTS, D_MODEL), f32)
    peer_v = (PEER_HEADS ** -0.5) * jax.random.normal(ks[11], (DEPTH, PEER_EXPERTS, D_MODEL), f32)
    normf_g = 1.0 + 0.05 * jax.random.normal(ks[12], (D_MODEL,), f32)
    return {"x": x, "norm1_g": norm1_g, "w_in": w_in, "b_forget": b_forget, "w_out": w_out,
            "norm2_g": norm2_g, "peer_wq": peer_wq, "peer_subkeys": peer_subkeys,
            "peer_u": peer_u, "peer_v": peer_v, "normf_g": normf_g}


def reference(x, norm1_g, w_in, b_forget, w_out, norm2_g, peer_wq, peer_subkeys, peer_u, peer_v, normf_g):
    h = x
    for layer in range(DEPTH):
        xn = rms_norm(h, norm1_g[layer])
        proj = xn @ w_in[layer]
        o0 = 3 * FOX_WIDTH
        o1 = o0 + N_FOX_HEADS
        fq = split_heads(proj[..., :FOX_WIDTH], N_FOX_HEADS)
        fk = split_heads(proj[..., FOX_WIDTH:2 * FOX_WIDTH], N_FOX_HEADS)
        fv = split_heads(proj[..., 2 * FOX_WIDTH:o0], N_FOX_HEADS)
        log_f = jax.nn.log_sigmoid(proj[..., o0:o1].astype(jnp.float32)
                                   + b_forget[layer].astype(jnp.float32)).transpose(0, 2, 1)
        dq = split_heads(proj[..., o1:o1 + DIL_WIDTH], N_DIL_HEADS)
        dk = split_heads(proj[..., o1 + DIL_WIDTH:o1 + 2 * DIL_WIDTH], N_DIL_HEADS)
        dv = split_heads(proj[..., o1 + 2 * DIL_WIDTH:o1 + 3 * DIL_WIDTH], N_DIL_HEADS)
        fox_out = fox_attention(fq, fk, fv, log_f)
        dil_out = dilated_attention(dq, dk, dv)
        mixed = jnp.concatenate([merge_heads(fox_out), merge_heads(dil_out)], axis=-1)
        h = h + mixed @ w_out[layer]
        xn2 = rms_norm(h, norm2_g[layer])
        h = h + peer_ffn(xn2, peer_wq[layer], peer_subkeys[layer], peer_u[layer], peer_v[layer])
    return rms_norm(h, normf_g)
```

```python
import numpy as np
import ml_dtypes
from contextlib import ExitStack
import concourse.bass as bass
import concourse.mybir as mybir
from concourse.bass_utils import run_bass_kernel_spmd

F32 = mybir.dt.float32
BF16 = mybir.dt.bfloat16
I32 = mybir.dt.int32
U32 = mybir.dt.uint32
AF = mybir.ActivationFunctionType
ALU = mybir.AluOpType
AX = mybir.AxisListType

LIM = 20000
NDMA = 40
NPOOL = 24
ENGS = ("pe", "act", "dve", "pool", "sp")


class Buf:
    def __init__(self, t=None, name=""):
        self.t = t
        self.name = name
        self.w = {}
        self.r = {}

    def __getitem__(self, k):
        return self.t[k]


class Ctx:
    def __init__(self, nc, es):
        self.nc = nc
        self.es = es
        self.root_es = es
        self.ops = {e: [] for e in ENGS}
        self.cnt = {e: 0 for e in ENGS}
        self.esems = {e: [] for e in ENGS}
        self.waited = {e: {} for e in ENGS}
        self.dsems = [es.enter_context(nc.semaphore("dq%d" % i)) for i in range(NDMA + NPOOL)]
        self.dvals = [0] * (NDMA + NPOOL)
        self.drr = 0
        self.prr = 0
        self.nsb = 0
        self.psems = {}
        self.pgen = {}

    def sb(self, shape, dtype, name=None):
        self.nsb += 1
        name = name or "sb%d" % self.nsb
        t = self.es.enter_context(self.nc.sbuf_tensor(name, list(shape), dtype))
        return Buf(t, name)

    def ps(self, shape, dtype, name=None):
        self.nsb += 1
        name = name or "ps%d" % self.nsb
        t = self.es.enter_context(self.nc.psum_tensor(name, list(shape), dtype))
        return Buf(t, name)

    def _esem(self, eng, ep):
        while len(self.esems[eng]) <= ep:
            self.esems[eng].append(
                self.root_es.enter_context(self.nc.semaphore("e_%s_%d" % (eng, len(self.esems[eng]))))
            )
        return self.esems[eng][ep]

    def _wait(self, eng, tok):
        key = tok[:2]
        v = tok[2]
        if self.waited[eng].get(key, 0) >= v:
            return
        self.waited[eng][key] = v
        if tok[0] == "e":
            ep, vv = divmod(v - 1, LIM)
            sem = self._esem(tok[1], ep)
            val = vv + 1
        elif tok[0] == "p":
            sem = self.psems[tok[1]]
            val = 16
        else:
            sem = self.dsems[tok[1]]
            val = v
        self.ops[eng].append(lambda e, s=sem, val=val: e.wait_ge(s, val))

    def _deps(self, eng, reads, writes, partial):
        toks = []
        for b in reads:
            toks.extend(t for (t, _p) in b.w.values())
        for b in writes:
            for (t, p_) in b.w.values():
                if partial and p_:
                    continue
                toks.append(t)
            toks.extend(b.r.values())
        for t in toks:
            if t[0] == "e" and t[1] == eng and eng == "pe":
                continue
            self._wait(eng, t)

    def op(self, eng, fn, reads=(), writes=(), partial=False, pwrites=()):
        self._deps(eng, reads, writes, partial)
        if pwrites:
            self._deps(eng, (), pwrites, True)
        self.cnt[eng] += 1
        n = self.cnt[eng]
        ep, vv = divmod(n - 1, LIM)
        sem = self._esem(eng, ep)
        self.ops[eng].append(lambda e, fn=fn, sem=sem: fn(e).then_inc(sem, 1))
        tok = ("e", eng, n)
        for b in pwrites:
            b.w[tok[:2]] = (tok, True)
        for b in writes:
            if partial:
                b.w[tok[:2]] = (tok, True)
            else:
                b.w = {tok[:2]: (tok, False)}
                b.r = {}
        for b in reads:
            b.r[tok[:2]] = tok
        return tok

    def dma(self, q, fn, reads=(), writes=(), partial=False):
        self._deps(q, reads, writes, partial)
        if q == "pool":
            k = NDMA + self.prr
            self.prr = (self.prr + 1) % NPOOL
        else:
            k = self.drr
            self.drr = (self.drr + 1) % NDMA
        if self.dvals[k] > 0:
            self._wait(q, ("d", k, self.dvals[k]))
        self.dvals[k] += 16
        v = self.dvals[k]
        sem = self.dsems[k]
        self.ops[q].append(lambda e, fn=fn, sem=sem: fn(e).then_inc(sem, 16))
        tok = ("d", k, v)
        for b in writes:
            if partial:
                b.w[tok[:2]] = (tok, True)
            else:
                b.w = {tok[:2]: (tok, False)}
                b.r = {}
        for b in reads:
            b.r[tok[:2]] = tok
        return tok

    def dma_pool(self, fn, dst, reads=()):
        return self.dma("pool", fn, reads=reads, writes=[dst])

    def finish(self, bufs, eng="sp"):
        for b in bufs:
            for (t, _p) in b.w.values():
                self._wait(eng, t)

    def barrier(self):
        for e in ENGS:
            for x in ENGS:
                if x != e and self.cnt[x] > 0:
                    self._wait(e, ("e", x, self.cnt[x]))
            for k in range(NDMA + NPOOL):
                if self.dvals[k] > 0:
                    self._wait(e, ("d", k, self.dvals[k]))

    def phase(self):
        return _Phase(self)

    def emit(self):
        nc = self.nc
        ops = self.ops
        self.ops = {e: [] for e in ENGS}
        self._emit(ops)

    def _emit(self, ops_):
        nc = self.nc
        self.ops_emit = ops_
        with nc.Block() as block:
            @block.tensor
            def _(e):
                for f in self.ops_emit["pe"]:
                    f(e)

            @block.scalar
            def _(e):
                for f in self.ops_emit["act"]:
                    f(e)

            @block.vector
            def _(e):
                for f in self.ops_emit["dve"]:
                    f(e)

            @block.gpsimd
            def _(e):
                for f in self.ops_emit["pool"]:
                    f(e)

            @block.sync
            def _(e):
                for f in self.ops_emit["sp"]:
                    f(e)


class _Phase:
    def __init__(self, c):
        self.c = c

    def __enter__(self):
        self.prev = self.c.es
        self.st = ExitStack()
        self.st.__enter__()
        self.c.es = self.st
        return self

    def __exit__(self, *a):
        if a[0] is None:
            self.c.barrier()
            self.c.emit()
        self.c.es = self.prev
        return self.st.__exit__(*a)

D = 1024
NH = 8
HD = 64
WIN = 3080
MASKW = 2944
NEXP = 16384


def _dil_mask_table():
    kl = np.arange(128)[:, None]
    ci = np.arange(MASKW)[None, :]
    delta = ci - 384 - kl
    ok = delta >= 0
    mult = (ok & (delta <= 128)).astype(np.float64)
    mult = mult + (ok & (delta <= 512) & (delta % 4 == 0))
    mult = mult + (ok & (delta <= 2048) & (delta % 16 == 0))
    slopes = 2.0 ** (-8.0 * np.arange(1, NH + 1) / NH)
    F = np.zeros((128, NH, MASKW), dtype=np.float64)
    for h in range(NH):
        F[:, h, :] = mult * np.exp(-slopes[h] * np.maximum(delta, 0))
    return np.ascontiguousarray(F.reshape(128, NH * MASKW).astype(ml_dtypes.bfloat16))


def _consts():
    c = {}
    c["c_identb"] = np.eye(128, dtype=np.float32).astype(ml_dtypes.bfloat16)
    c["c_identf"] = np.eye(128, dtype=np.float32)
    k = np.arange(128)
    c["c_tri"] = (k[:, None] <= k[None, :]).astype(np.float32)
    c["c_trib"] = (k[None, :] >= k[:, None]).astype(np.float32).astype(ml_dtypes.bfloat16)
    c["c_ones"] = np.ones((128, 128), dtype=np.float32)
    c["c_iota16"] = np.tile(np.arange(16, dtype=np.float32)[None, :], (128, 1))
    c["c_dmask"] = _dil_mask_table()
    return c


def build(T, dbg=False):
    consts_np = _consts()
    NT = T // 128
    NG = T // 512
    nc = bass.Bass("TRN2", target_bir_lowering=False)

    def din(name, shape, dt=F32):
        return nc.dram_tensor(name, list(shape), dt, kind="ExternalInput").ap()

    x = din("x", [T, D])
    g1T = din("g1T", [128, 8])
    w_in = din("w_in", [D, WIN])
    bfg = din("b_forget", [1, NH])
    w_out = din("w_out", [D, D])
    g2T = din("g2T", [128, 8])
    g2 = din("g2", [1, D])
    wq = din("peer_wq", [D, 2048])
    subk = din("peer_subkeys", [16, 128, 128])
    pu = din("peer_u", [NEXP, D])
    pv = din("peer_v", [NEXP, D])
    gf = din("normf_g", [1, D])
    c_identb = din("c_identb", [128, 128], BF16)
    c_identf = din("c_identf", [128, 128])
    c_tri = din("c_tri", [128, 128])
    c_trib = din("c_trib", [128, 128], BF16)
    c_ones = din("c_ones", [128, 128])
    c_iota16 = din("c_iota16", [128, 16])
    c_dmask = din("c_dmask", [128, NH * MASKW], BF16)
    y = nc.dram_tensor("y", [T, D], F32, kind="ExternalOutput").ap()

    skind = "ExternalOutput" if dbg else "Internal"
    qkT = nc.dram_tensor("s_qkT", [2048, T], BF16, kind=skind).ap()
    vaug = nc.dram_tensor("s_vaug", [2, T, NH * 128], BF16, kind=skind).ap()
    mixT = nc.dram_tensor("s_mixT", [D, T], BF16, kind=skind).ap()
    uv = nc.dram_tensor("s_uv", [NEXP, 2 * D], BF16, kind="Internal").ap()
    uv_b = Buf(None)
    if dbg:
        d_L = nc.dram_tensor("d_L", [128, NT * NH], F32, kind="ExternalOutput").ap()
        d_hid = nc.dram_tensor("d_hid", [T, 128], F32, kind="ExternalOutput").ap()
        d_eid = nc.dram_tensor("d_eid", [T, 128], I32, kind="ExternalOutput").ap()
        d_gate = nc.dram_tensor("d_gate", [T, 128], F32, kind="ExternalOutput").ap()
        d_h = nc.dram_tensor("d_h", [T, D], F32, kind="ExternalOutput").ap()
    qk_b = [[Buf(None) for _ in range(NG)] for _ in range(16)]
    va_b = [[Buf(None) for _ in range(NT)] for _ in range(2)]
    mx_b = [[Buf(None) for _ in range(NG)] for _ in range(16)]
    y_b = [Buf(None) for _ in range(NT)]
    dbg_b = Buf(None)

    with ExitStack() as es:
        c = Ctx(nc, es)
        identb = c.sb([128, 128], BF16)
        identf = c.sb([128, 128], F32)
        tri = c.sb([128, 128], F32)
        trib = c.sb([128, 128], BF16)
        ones = c.sb([128, 128], F32)
        iota16 = c.sb([128, 16], F32)
        g1t = c.sb([128, 8], F32)
        g2t = c.sb([128, 8], F32)
        g2bc = c.sb([128, D], F32)
        gfbc = c.sb([128, D], F32)
        bfbc = c.sb([128, NH], F32)
        eps = c.sb([128, 1], F32)
        for t_, src in ((identb, c_identb), (identf, c_identf), (tri, c_tri), (trib, c_trib),
                        (ones, c_ones), (iota16, c_iota16), (g1t, g1T), (g2t, g2T)):
            c.dma("sp", lambda e, t_=t_, src=src: e.dma_start(out=t_[:], in_=src), writes=[t_])
        c.dma("sp", lambda e: e.dma_start(out=g2bc[:], in_=g2.partition_broadcast(128)), writes=[g2bc])
        c.dma("sp", lambda e: e.dma_start(out=gfbc[:], in_=gf.partition_broadcast(128)), writes=[gfbc])
        c.dma("sp", lambda e: e.dma_start(out=bfbc[:], in_=bfg.partition_broadcast(128)), writes=[bfbc])
        c.op("pool", lambda e: e.memset(eps[:], 1e-6), writes=[eps])

        PS = [c.ps([128, 512], F32, name="bank%d" % i) for i in range(8)]

        xts = [c.sb([128, D], F32, name="xt%d" % i) for i in range(2)]
        junk = c.sb([128, D], F32, name="junk")
        xss = [c.sb([128, D], BF16, name="xs%d" % i) for i in range(2)]
        ssq = c.sb([128, 1], F32)
        rstd = c.sb([128, 1], F32)
        Lc = c.sb([128, NT, NH], F32, name="Lc")
        carry = c.sb([128, NT, NH], F32, name="carry")
        tot = c.sb([128, NT, NH], F32, name="tot")
        biasq = c.sb([128, NG, NT, NH], F32, name="biasq")
        with c.phase():
            winb = c.sb([128, 8, WIN], BF16, name="winb")
            wstage = [c.sb([128, WIN], F32, name="wst%d" % i) for i in range(2)]
            for cc in range(8):
                st = wstage[cc % 2]
                c.dma("sp", lambda e, st=st, cc=cc: e.dma_start(out=st[:], in_=w_in[cc * 128:(cc + 1) * 128, :]), writes=[st])
                c.op("dve", lambda e, st=st, cc=cc: e.tensor_scalar(out=winb[:, cc, :], in0=st[:], scalar1=g1t[:, cc:cc + 1],
                                                                     scalar2=None, op0=ALU.mult),
                     reads=[st, g1t], writes=[winb], partial=True)

            xnT = c.sb([128, 8, 512], BF16, name="xnT")
            lf_all = c.sb([128, NT, NH], F32, name="lf_all")
            vst = [c.sb([128, 2, NH, 128], BF16, name="vst%d" % i) for i in range(2)]
            for v_ in vst:
                c.op("pool", lambda e, v_=v_: e.memset(v_[:], 1.0), writes=[v_])
            qst = [c.sb([128, 512], BF16, name="qst%d" % i) for i in range(3)]
            zt = c.sb([128, NH], F32)
            et = c.sb([128, NH], F32)
            qcols = [0, 512, 1544, 2056]
            vcols = [1024, 2568]
            nq = 0
            for g in range(NG):
                for j in range(4):
                    i = g * 4 + j
                    xt = xts[i % 2]
                    xs = xss[i % 2]
                    c.dma("sp", lambda e, xt=xt, i=i: e.dma_start(out=xt[:], in_=x[i * 128:(i + 1) * 128, :]), writes=[xt])
                    c.op("act", lambda e, xt=xt: e.activation(out=junk[:], in_=xt[:], func=AF.Square, accum_out=ssq[:]),
                         reads=[xt], writes=[junk, ssq])
                    c.op("act", lambda e: e.activation(out=rstd[:], in_=ssq[:], func=AF.Sqrt, scale=1.0 / D, bias=eps[:]),
                         reads=[ssq, eps], writes=[rstd])
                    c.op("dve", lambda e: e.reciprocal(out=rstd[:], in_=rstd[:]), reads=[rstd], writes=[rstd])
                    c.op("act", lambda e, xt=xt, xs=xs: e.activation(out=xs[:], in_=xt[:], func=AF.Copy, scale=rstd[:]),
                         reads=[xt, rstd], writes=[xs])
                    pT = PS[0]
                    pTv = pT.t[:].bitcast(BF16)
                    for k in range(8):
                        c.op("pe", lambda e, k=k, xs=xs, pTv=pTv: e.transpose(out=pTv[:, k * 128:(k + 1) * 128],
                                                                              in_=xs[:, k * 128:(k + 1) * 128], identity=identb[:]),
                             reads=[xs, identb], writes=[pT], partial=True)
                    c.op("dve", lambda e, j=j, pTv=pTv: e.tensor_copy(out=xnT[:, :, j * 128:(j + 1) * 128],
                                                                       in_=pTv.rearrange("p (k t) -> p k t", k=8)),
                         reads=[pT], writes=[xnT], partial=True)
                    vs = vst[i % 2]
                    for m in range(2):
                        pv_ = PS[1 + m]
                        for k in range(8):
                            c.op("pe", lambda e, k=k, j=j, m=m, pv_=pv_: e.matmul(pv_[:], lhsT=xnT[:, k, j * 128:(j + 1) * 128],
                                                                                  rhs=winb[:, k, vcols[m]:vcols[m] + 512],
                                                                                  start=(k == 0), stop=(k == 7)),
                                 reads=[xnT, winb], writes=[pv_], partial=True)
                        c.op("act", lambda e, m=m, vs=vs, pv_=pv_: e.copy(out=vs[:, m, :, 0:64],
                                                                           in_=pv_[:].rearrange("p (h d) -> p h d", h=NH)),
                             reads=[pv_], writes=[vs], partial=True)
                    pf = PS[3]
                    for k in range(8):
                        c.op("pe", lambda e, k=k, j=j: e.matmul(pf[:, 0:NH], lhsT=xnT[:, k, j * 128:(j + 1) * 128],
                                                                rhs=winb[:, k, 1536:1536 + NH], start=(k == 0), stop=(k == 7)),
                             reads=[xnT, winb], writes=[pf], partial=True)
                    c.op("dve", lambda e: e.tensor_tensor(out=zt[:], in0=pf[:, 0:NH], in1=bfbc[:], op=ALU.add),
                         reads=[pf, bfbc], writes=[zt])
                    c.op("act", lambda e: e.activation(out=et[:], in_=zt[:], func=AF.Exp, scale=-1.0), reads=[zt], writes=[et])
                    c.op("act", lambda e, i=i: e.activation(out=lf_all[:, i, :], in_=et[:], func=AF.Ln, bias=1.0),
                         reads=[et], writes=[lf_all], partial=True)
                    for m in range(2):
                        c.dma("pool", lambda e, m=m, vs=vs, i=i: e.dma_start(
                            out=vaug[m, i * 128:(i + 1) * 128, :], in_=vs[:, m, :, :].rearrange("p h d -> p (h d)")),
                            reads=[vs], writes=[va_b[m][i]])
                for f in range(16):
                    cb = qcols[f // 4] + (f % 4) * 128
                    pq = PS[4 + (f % 2)]
                    for k in range(8):
                        c.op("pe", lambda e, k=k, cb=cb, pq=pq: e.matmul(pq[:], lhsT=winb[:, k, cb:cb + 128], rhs=xnT[:, k, :],
                                                                         start=(k == 0), stop=(k == 7)),
                             reads=[winb, xnT], writes=[pq], partial=True)
                    qs = qst[nq % 3]
                    nq += 1
                    eng = "act" if f % 2 == 0 else "dve"
                    if eng == "act":
                        c.op("act", lambda e, qs=qs, pq=pq: e.copy(out=qs[:], in_=pq[:]), reads=[pq], writes=[qs])
                    else:
                        c.op("dve", lambda e, qs=qs, pq=pq: e.tensor_copy(out=qs[:], in_=pq[:]), reads=[pq], writes=[qs])
                    c.dma("pool", lambda e, qs=qs, f=f, g=g: e.dma_start(out=qkT[f * 128:(f + 1) * 128, g * 512:(g + 1) * 512], in_=qs[:]),
                          reads=[qs], writes=[qk_b[f][g]])

            pc = PS[6]
            pt_ = PS[7]
            NTH = NT * NH
            lf2 = lf_all[:].rearrange("p t h -> p (t h)")
            c.op("pe", lambda e: e.matmul(pc[:, 0:NTH], lhsT=tri[:], rhs=lf2, start=True, stop=True), reads=[tri, lf_all], writes=[pc])
            c.op("pe", lambda e: e.matmul(pt_[:, 0:NTH], lhsT=ones[:], rhs=lf2, start=True, stop=True), reads=[ones, lf_all], writes=[pt_])
            c.op("dve", lambda e: e.tensor_copy(out=tot[:].rearrange("p t h -> p (t h)"), in_=pt_[:, 0:NTH]), reads=[pt_], writes=[tot])
            c.op("dve", lambda e: e.memset(carry[:, 0, :], 0.0), writes=[carry])
            for i in range(1, NT):
                c.op("dve", lambda e, i=i: e.tensor_tensor(out=carry[:, i, :], in0=carry[:, i - 1, :], in1=tot[:, i - 1, :], op=ALU.add),
                     reads=[carry, tot], writes=[carry])
            c.op("dve", lambda e: e.tensor_tensor(out=Lc[:].rearrange("p t h -> p (t h)"), in0=pc[:, 0:NTH],
                                                  in1=carry[:].rearrange("p t h -> p (t h)"), op=ALU.add),
                 reads=[pc, carry], writes=[Lc])
            for qb in range(NG):
                c.op("dve", lambda e, qb=qb: e.tensor_tensor(
                    out=biasq[:, qb, :, :], in0=Lc[:],
                    in1=carry[:, 4 * qb:4 * qb + 1, :].to_broadcast([128, NT, NH]), op=ALU.subtract),
                    reads=[Lc, carry], writes=[biasq], partial=True)
            if dbg:
                c.dma("sp", lambda e: e.dma_start(out=d_L, in_=Lc[:].rearrange("p t h -> p (t h)")), reads=[Lc], writes=[dbg_b], partial=True)

        with c.phase():
            cast_q = []
            for tbl, c0_ in ((pu, 0), (pv, D)):
                for r0 in range(0, NEXP, 1024):
                    cast_q.append(lambda tbl=tbl, c0_=c0_, r0=r0: c.dma(
                        "pool", lambda e: e.dma_start(out=uv[r0:r0 + 1024, c0_:c0_ + D], in_=tbl[r0:r0 + 1024, :]),
                        writes=[uv_b], partial=True))

            Vaug = c.sb([128, NT, NH, 128], BF16, name="Vaug")
            dmask = c.sb([128, NH, MASKW], BF16, name="dmask")
            c.dma("sp", lambda e: e.dma_start(out=dmask[:].rearrange("p h w -> p (h w)"), in_=c_dmask), writes=[dmask])
            while cast_q:
                cast_q.pop(0)()
            QT = [c.sb([128, T], BF16, name="QT%d" % i) for i in range(2)]
            KT = [c.sb([128, T], BF16, name="KT%d" % i) for i in range(2)]
            c.op("dve", lambda e: e.memset(QT[0][64:128, :], 0.0), writes=[QT[0]])
            c.op("dve", lambda e: e.memset(QT[1][0:64, :], 0.0), writes=[QT[1]])
            NPT = 9
            ptb = [c.sb([128, 512], BF16, name="ptb%d" % i) for i in range(NPT)]
            rz = c.sb([128, 512], F32, name="rz")
            mxs = [c.sb([64, 512], BF16, name="mxs%d" % i) for i in range(2)]
            SB_ = [PS[0], PS[1], PS[4], PS[5], PS[6], PS[7]]
            OB_ = [PS[2], PS[3]]
            dm_np = consts_np["c_dmask"].astype(np.float32).reshape(128, NH, MASKW)

            jobs = [(m, h) for m in range(2) for h in range(NH)]
            pairs = []
            ngrp = 0
            for (m, h) in jobs:
                for qb in range(NG):
                    kb0 = 0 if m == 0 else max(0, 4 * qb - 16)
                    kbl = 4 * qb + 3
                    grp = []
                    for kb in range(kb0, kbl + 1):
                        Dd = 4 * qb - kb
                        c0 = max(0, -128 * Dd)
                        c1 = 512 if m == 0 else min(512, 2176 - 128 * Dd)
                        ci0 = 128 * Dd + c0 + 384
                        if m == 1 and not np.any(dm_np[:, h, ci0:ci0 + (c1 - c0)]):
                            continue
                        grp.append(dict(m=m, h=h, qb=qb, kb=kb, Dd=Dd, c0=c0, c1=c1, ci0=ci0, g=ngrp))
                    grp[0]["first"] = True
                    grp[-1]["last"] = True
                    pairs.extend(grp)
                    ngrp += 1

            def load_job(j):
                m, h = jobs[j]
                if h == 0:
                    for i in range(NT):
                        c.dma("sp", lambda e, m=m, i=i: e.dma_start(out=Vaug[:, i, :, :].rearrange("p h d -> p (h d)"),
                                                                     in_=vaug[m, i * 128:(i + 1) * 128, :]),
                              reads=[va_b[m][i]], writes=[Vaug], partial=True)
                qt_, kt_ = QT[j % 2], KT[j % 2]
                pb = (h % 2) * 64
                qrow = m * 1024 + h * 64
                krow = m * 1024 + 512 + (h // 2) * 128
                qdeps = [qk_b[qrow // 128][g] for g in range(NG)]
                kdeps = [qk_b[krow // 128][g] for g in range(NG)]
                c.dma("sp", lambda e, qt_=qt_, qrow=qrow, pb=pb: e.dma_start(out=qt_[pb:pb + 64, :], in_=qkT[qrow:qrow + 64, :]),
                      reads=qdeps, writes=[qt_], partial=True)
                c.dma("sp", lambda e, kt_=kt_, krow=krow: e.dma_start(out=kt_[:], in_=qkT[krow:krow + 128, :]), reads=kdeps, writes=[kt_])

            def emit_S(n):
                p = pairs[n]
                j = p["m"] * NH + p["h"]
                qt_, kt_ = QT[j % 2], KT[j % 2]
                psb = SB_[n % 6]
                kb, qb, c0, c1 = p["kb"], p["qb"], p["c0"], p["c1"]
                c.op("pe", lambda e: e.matmul(psb[:, c0:c1], lhsT=kt_[:, kb * 128:(kb + 1) * 128],
                                              rhs=qt_[:, qb * 512 + c0:qb * 512 + c1], start=True, stop=True),
                     reads=[kt_, qt_], writes=[psb])

            def emit_exp(n):
                p = pairs[n]
                m, h, kb, qb, c0, c1, Dd, ci0 = p["m"], p["h"], p["kb"], p["qb"], p["c0"], p["c1"], p["Dd"], p["ci0"]
                psb = SB_[n % 6]
                pt = ptb[n % NPT]
                if m == 0:
                    c.op("act", lambda e: e.activation(out=pt[:, c0:c1], in_=psb[:, c0:c1], func=AF.Exp, scale=0.125,
                                                       bias=biasq[:, qb, kb, h:h + 1]),
                         reads=[psb, biasq], writes=[pt])
                    if Dd <= 0:
                        c.op("dve", lambda e: e.tensor_tensor(out=pt[:, c0:c0 + 128], in0=pt[:, c0:c0 + 128], in1=trib[:], op=ALU.mult),
                             reads=[pt, trib], writes=[pt])
                else:
                    c.op("act", lambda e: e.activation(out=pt[:, c0:c1], in_=psb[:, c0:c1], func=AF.Exp, scale=0.125),
                         reads=[psb], writes=[pt])
                    c.op("dve", lambda e: e.tensor_tensor(out=pt[:, c0:c1], in0=pt[:, c0:c1], in1=dmask[:, h, ci0:ci0 + (c1 - c0)], op=ALU.mult),
                         reads=[pt, dmask], writes=[pt])

            def emit_pv(n):
                p = pairs[n]
                m, h, kb, qb, c0, c1 = p["m"], p["h"], p["kb"], p["qb"], p["c0"], p["c1"]
                pt = ptb[n % NPT]
                po = OB_[p["g"] % 2]
                c.op("pe", lambda e: e.matmul(po[:, c0:c1], lhsT=Vaug[:, kb, h, :], rhs=pt[:, c0:c1],
                                              start=bool(p.get("first")), stop=bool(p.get("last")), skip_group_check=True),
                     reads=[Vaug, pt], writes=[po], partial=True)
                if p.get("last"):
                    mx = mxs[p["g"] % 2]
                    if m == 1:
                        c.op("act", lambda e: e.activation(out=rz[64:128, :], in_=po[64:128, :], func=AF.Ln), reads=[po], writes=[rz])
                        c.op("act", lambda e: e.activation(out=rz[64:128, :], in_=rz[64:128, :], func=AF.Exp, scale=-1.0), reads=[rz], writes=[rz])
                    else:
                        c.op("dve", lambda e: e.reciprocal(out=rz[64:128, :], in_=po[64:128, :]), reads=[po], writes=[rz])
                    c.op("dve", lambda e: e.tensor_tensor(out=mx[:], in0=po[0:64, :], in1=rz[64:128, :], op=ALU.mult),
                         reads=[po, rz], writes=[mx])
                    mrow = m * 512 + h * 64
                    c.dma("sp", lambda e: e.dma_start(out=mixT[mrow:mrow + 64, qb * 512:(qb + 1) * 512], in_=mx[:]),
                          reads=[mx], writes=[mx_b[m * 8 + h][qb]])

            BS = 3
            for mm in range(2):
                idxs = [n for n in range(len(pairs)) if pairs[n]["m"] == mm]
                batches = [idxs[i_:i_ + BS] for i_ in range(0, len(idxs), BS)]
                NB = len(batches)
                loaded = set()
                for k in range(NB + 2):
                    if k < NB:
                        for n in batches[k]:
                            j = pairs[n]["m"] * NH + pairs[n]["h"]
                            if j not in loaded:
                                load_job(j)
                                loaded.add(j)
                                if (j + 1) < (mm + 1) * NH and (j + 1) not in loaded:
                                    load_job(j + 1)
                                    loaded.add(j + 1)
                            emit_S(n)
                    if 1 <= k <= NB:
                        for n in batches[k - 1]:
                            emit_exp(n)
                    if 2 <= k <= NB + 1:
                        for n in batches[k - 2]:
                            emit_pv(n)

            while cast_q:
                cast_q.pop(0)()

        with c.phase():
            woutb = c.sb([128, 8, D], BF16, name="woutb")
            Ws = c.sb([128, 8, 2048], BF16, name="Ws")
            with c.phase():
                wstage = [c.sb([128, 2048], F32, name="wst4_%d" % i) for i in range(2)]
                for cc in range(8):
                    st = wstage[cc % 2]
                    c.dma("sp", lambda e, st=st, cc=cc: e.dma_start(out=st[:, 0:D], in_=w_out[cc * 128:(cc + 1) * 128, :]), writes=[st])
                    c.op("dve", lambda e, st=st, cc=cc: e.tensor_copy(out=woutb[:, cc, :], in_=st[:, 0:D]), reads=[st], writes=[woutb], partial=True)
                wqT = c.sb([128, 16, D], BF16, name="wqT")
                skT = c.sb([128, 16, 128], BF16, name="skT")
                nb_ = 0
                for cc in range(8):
                    st = wstage[cc % 2]
                    c.dma("sp", lambda e, st=st, cc=cc: e.dma_start(out=st[:, 0:2048], in_=wq[cc * 128:(cc + 1) * 128, :]), writes=[st])
                    for q4 in range(4):
                        pb = PS[nb_ % 2]
                        nb_ += 1
                        for r in range(4):
                            hp = q4 * 4 + r
                            c.op("pe", lambda e, pb=pb, r=r, hp=hp, st=st: e.transpose(out=pb[:, r * 128:(r + 1) * 128],
                                                                                     in_=st[:, hp * 128:(hp + 1) * 128], identity=identf[:]),
                                 reads=[st, identf], writes=[pb], partial=True)
                        c.op("act", lambda e, pb=pb, q4=q4, cc=cc: e.copy(out=wqT[:, q4 * 4:(q4 + 1) * 4, cc * 128:(cc + 1) * 128],
                                                                          in_=pb[:].rearrange("p (r d) -> p r d", r=4)),
                             reads=[pb], writes=[wqT], partial=True)
                skst = c.sb([128, 16, 128], F32, name="skst")
                c.dma("sp", lambda e: e.dma_start(out=skst[:], in_=subk.rearrange("g k j -> k g j")), writes=[skst])
                for q4 in range(4):
                    pb = PS[nb_ % 2]
                    nb_ += 1
                    for r in range(4):
                        hp = q4 * 4 + r
                        c.op("pe", lambda e, pb=pb, r=r, hp=hp: e.transpose(out=pb[:, r * 128:(r + 1) * 128], in_=skst[:, hp, :], identity=identf[:]),
                             reads=[skst, identf], writes=[pb], partial=True)
                    c.op("act", lambda e, pb=pb, q4=q4: e.copy(out=skT[:, q4 * 4:(q4 + 1) * 4, :], in_=pb[:].rearrange("p (r k) -> p r k", r=4)),
                         reads=[pb], writes=[skT], partial=True)
                for cc in range(8):
                    for q4 in range(4):
                        pb = PS[nb_ % 2]
                        nb_ += 1
                        for r in range(4):
                            hp = q4 * 4 + r
                            c.op("pe", lambda e, pb=pb, r=r, hp=hp, cc=cc: e.matmul(pb[:, r * 128:(r + 1) * 128], lhsT=wqT[:, hp, cc * 128:(cc + 1) * 128],
                                                                                    rhs=skT[:, hp, :], start=True, stop=True),
                                 reads=[wqT, skT], writes=[pb], partial=True)
                        c.op("dve", lambda e, pb=pb, q4=q4, cc=cc: e.tensor_scalar(out=Ws[:, cc, q4 * 512:(q4 + 1) * 512], in0=pb[:],
                                                                                   scalar1=g2t[:, cc:cc + 1], scalar2=None, op0=ALU.mult),
                             reads=[pb, g2t], writes=[Ws], partial=True)

            NR = 12
            mts = [c.sb([128, 8, 128], BF16, name="mt%d" % i) for i in range(2)]
            hts = [c.sb([128, D], F32, name="ht%d" % i) for i in range(2)]
            xn2s = [c.sb([128, D], BF16, name="xn2_%d" % i) for i in range(2)]
            xn2T = c.sb([128, 8, 128], BF16, name="xn2T")
            sc = c.sb([128, 2048], F32, name="sc")
            top = c.sb([128, 16, 16], F32, name="top")
            idx = c.sb([128, 16, 16], U32, name="idx")
            idxf = c.sb([128, 16, 16], F32, name="idxf")
            cand = c.sb([128, NH, 256], F32, name="cand")
            ctop = c.sb([128, NH, 16], F32, name="ctop")
            cpos = c.sb([128, NH, 16], U32, name="cpos")
            ci_ = c.sb([128, 128], U32, name="ci_")
            cj_ = c.sb([128, 128], U32, name="cj_")
            cif = c.sb([128, 128], F32, name="cif")
            cjf = c.sb([128, 128], F32, name="cjf")
            oh = c.sb([128, 128, 16], F32, name="oh")
            i1s = c.sb([128, 128], F32, name="i1s")
            i2s = c.sb([128, 128], F32, name="i2s")
            eidf = c.sb([128, 128], F32, name="eidf")
            eids = [c.sb([128, 128], I32, name="eid%d" % i) for i in range(2)]
            gates = [c.sb([128, NH, 16], F32, name="gate%d" % i) for i in range(2)]
            zs = c.sb([128, NH], F32, name="zs")
            hid = c.sb([128, 128], F32, name="hid")
            gel = c.sb([128, 128], F32, name="gel")
            acol = c.sb([128, 128], F32, name="acol")
            uvgs = [c.sb([128, 2 * D], BF16, name="uvg%d" % i) for i in range(NR)]
            dgs = [c.sb([128, 128], BF16, name="dg%d" % i) for i in range(4)]
            junkbs = [c.sb([128, D], BF16, name="junkb%d" % i) for i in range(4)]
            h2 = c.sb([128, D], F32, name="h2")
            yo = [c.sb([128, D], F32, name="yo%d" % i) for i in range(2)]
            ssq2 = c.sb([128, 1], F32)
            rstd2 = c.sb([128, 1], F32)
            ssq3 = c.sb([128, 1], F32)
            rstd3 = c.sb([128, 1], F32)

            hid_c = [Buf(hid.t, "hid_c%d" % i_) for i_ in range(128)]
            gel_c = [Buf(gel.t, "gel_c%d" % i_) for i_ in range(128)]
            acol_c = [Buf(acol.t, "acol_c%d" % i_) for i_ in range(128)]
            sc_g = [Buf(sc.t, "sc_g%d" % i_) for i_ in range(16)]
            top_g = [Buf(top.t, "top_g%d" % i_) for i_ in range(16)]
            idx_g = [Buf(idx.t, "idx_g%d" % i_) for i_ in range(16)]
            cand_g = [Buf(cand.t, "cand_g%d" % i_) for i_ in range(NH)]
            ctop_g = [Buf(ctop.t, "ctop_g%d" % i_) for i_ in range(NH)]
            cpos_g = [Buf(cpos.t, "cpos_g%d" % i_) for i_ in range(NH)]

            def stageA(i):
                q = []

                def o(*a, **k):
                    q.append(lambda: c.op(*a, **k))

                def dm(*a, **k):
                    q.append(lambda: c.dma(*a, **k))

                def top16_multi(specs):
                    for (src_ap, val_ap, idx_ap, src_b, val_b, idx_b) in specs:
                        o("dve", lambda e, src_ap=src_ap, val_ap=val_ap: e.max(out=val_ap[:, 0:8], in_=src_ap), reads=[src_b], writes=[val_b])
                    for (src_ap, val_ap, idx_ap, src_b, val_b, idx_b) in specs:
                        o("dve", lambda e, src_ap=src_ap, val_ap=val_ap, idx_ap=idx_ap: e.max_index(out=idx_ap[:, 0:8], in_max=val_ap[:, 0:8], in_values=src_ap),
                          reads=[src_b, val_b], writes=[idx_b])
                    for (src_ap, val_ap, idx_ap, src_b, val_b, idx_b) in specs:
                        o("dve", lambda e, src_ap=src_ap, val_ap=val_ap: e.match_replace(out=src_ap, in_to_replace=val_ap[:, 0:8], in_values=src_ap, imm_value=-1e30),
                          reads=[val_b], writes=[src_b])
                    for (src_ap, val_ap, idx_ap, src_b, val_b, idx_b) in specs:
                        o("dve", lambda e, src_ap=src_ap, val_ap=val_ap: e.max(out=val_ap[:, 8:16], in_=src_ap), reads=[src_b], writes=[val_b], partial=True)
                    for (src_ap, val_ap, idx_ap, src_b, val_b, idx_b) in specs:
                        o("dve", lambda e, src_ap=src_ap, val_ap=val_ap, idx_ap=idx_ap: e.max_index(out=idx_ap[:, 8:16], in_max=val_ap[:, 8:16], in_values=src_ap),
                          reads=[src_b, val_b], writes=[idx_b], partial=True)

                g = i // 4
                par = i % 2
                mt, xt, ht, xs, xn2, eid, gate = mts[par], xts[par], hts[par], xss[par], xn2s[par], eids[par], gates[par]
                mdeps = [mx_b[hh][g] for hh in range(16)]
                dm("sp", lambda e: e.dma_start(out=mt[:], in_=mixT.rearrange("(c p) t -> p c t", p=128)[:, :, i * 128:(i + 1) * 128]),
                   reads=mdeps, writes=[mt])
                dm("sp", lambda e: e.dma_start(out=xt[:], in_=x[i * 128:(i + 1) * 128, :]), writes=[xt])
                for hf in range(2):
                    ph = PS[hf]
                    for k in range(8):
                        o("pe", lambda e, ph=ph, k=k, hf=hf: e.matmul(ph[:], lhsT=mt[:, k, :], rhs=woutb[:, k, hf * 512:(hf + 1) * 512],
                                                                        start=(k == 0), stop=(k == 7)),
                          reads=[mt, woutb], writes=[ph], partial=True)
                    o("dve", lambda e, ph=ph, hf=hf: e.tensor_tensor(out=ht[:, hf * 512:(hf + 1) * 512], in0=ph[:],
                                                                     in1=xt[:, hf * 512:(hf + 1) * 512], op=ALU.add),
                      reads=[ph, xt], writes=[ht], partial=True)
                o("act", lambda e: e.activation(out=junk[:], in_=ht[:], func=AF.Square, accum_out=ssq2[:]), reads=[ht], writes=[junk, ssq2])
                o("act", lambda e: e.activation(out=rstd2[:], in_=ssq2[:], func=AF.Sqrt, scale=1.0 / D, bias=eps[:]),
                  reads=[ssq2, eps], writes=[rstd2])
                o("dve", lambda e: e.reciprocal(out=rstd2[:], in_=rstd2[:]), reads=[rstd2], writes=[rstd2])
                o("act", lambda e: e.activation(out=xs[:], in_=ht[:], func=AF.Copy, scale=rstd2[:]), reads=[ht, rstd2], writes=[xs])
                o("dve", lambda e: e.scalar_tensor_tensor(out=xn2[:], in0=ht[:], scalar=rstd2[:], in1=g2bc[:], op0=ALU.mult, op1=ALU.mult),
                  reads=[ht, rstd2, g2bc], writes=[xn2])
                pT = PS[0]
                pTv = pT.t[:].bitcast(BF16)
                for k in range(8):
                    o("pe", lambda e, k=k: e.transpose(out=pTv[:, k * 128:(k + 1) * 128], in_=xs[:, k * 128:(k + 1) * 128], identity=identb[:]),
                      reads=[xs, identb], writes=[pT], partial=True)
                o("act", lambda e: e.copy(out=xn2T[:], in_=pTv.rearrange("p (k t) -> p k t", k=8)), reads=[pT], writes=[xn2T])
                for q4 in range(4):
                    pb = PS[4 + (q4 % 2)]
                    for k in range(8):
                        o("pe", lambda e, pb=pb, k=k, q4=q4: e.matmul(pb[:], lhsT=xn2T[:, k, :], rhs=Ws[:, k, q4 * 512:(q4 + 1) * 512],
                                                                      start=(k == 0), stop=(k == 7)),
                          reads=[xn2T, Ws], writes=[pb], partial=True)
                    o("act", lambda e, pb=pb, q4=q4: e.copy(out=sc[:, q4 * 512:(q4 + 1) * 512], in_=pb[:]), reads=[pb], writes=sc_g[4 * q4:4 * q4 + 4])
                top16_multi([(sc[:, gq * 128:(gq + 1) * 128], top[:, gq, :], idx[:, gq, :], sc_g[gq], top_g[gq], idx_g[gq]) for gq in range(16)])
                o("dve", lambda e: e.tensor_copy(out=idxf[:], in_=idx[:]), reads=idx_g, writes=[idxf])
                top4 = top[:].rearrange("p (h two) k -> p h two k", two=2)
                o("dve", lambda e: e.tensor_tensor(
                    out=cand[:].rearrange("p h (a b) -> p h a b", a=16),
                    in0=top4[:, :, 0, :].unsqueeze(3).to_broadcast([128, NH, 16, 16]),
                    in1=top4[:, :, 1, :].unsqueeze(2).to_broadcast([128, NH, 16, 16]), op=ALU.add),
                    reads=top_g, writes=cand_g)
                top16_multi([(cand[:, hh, :], ctop[:, hh, :], cpos[:, hh, :], cand_g[hh], ctop_g[hh], cpos_g[hh]) for hh in range(NH)])
                n_g0 = len(q)
                o("dve", lambda e: e.tensor_tensor(out=gate[:], in0=ctop[:], in1=ctop[:, :, 0:1].to_broadcast([128, NH, 16]), op=ALU.subtract),
                  reads=ctop_g, writes=[gate])
                o("act", lambda e: e.activation(out=gate[:], in_=gate[:], func=AF.Exp), reads=[gate], writes=[gate])
                o("dve", lambda e: e.tensor_reduce(out=zs[:], in_=gate[:], op=ALU.add, axis=AX.X), reads=[gate], writes=[zs])
                o("dve", lambda e: e.reciprocal(out=zs[:], in_=zs[:]), reads=[zs], writes=[zs])
                o("dve", lambda e: e.tensor_tensor(out=gate[:], in0=gate[:], in1=zs[:].unsqueeze(2).to_broadcast([128, NH, 16]), op=ALU.mult),
                  reads=[gate, zs], writes=[gate])
                n_g1 = len(q)
                cp2 = cpos[:].rearrange("p h k -> p (h k)")
                o("dve", lambda e: e.tensor_single_scalar(out=ci_[:], in_=cp2, scalar=4, op=ALU.logical_shift_right), reads=cpos_g, writes=[ci_])
                o("dve", lambda e: e.tensor_single_scalar(out=cj_[:], in_=cp2, scalar=15, op=ALU.bitwise_and), reads=cpos_g, writes=[cj_])
                o("dve", lambda e: e.tensor_copy(out=cif[:], in_=ci_[:]), reads=[ci_], writes=[cif])
                o("dve", lambda e: e.tensor_copy(out=cjf[:], in_=cj_[:]), reads=[cj_], writes=[cjf])
                idx4 = idxf[:].rearrange("p (h two) k -> p h two k", two=2)
                for (srcf, pp, dst) in ((cif, 0, i1s), (cjf, 1, i2s)):
                    o("dve", lambda e, srcf=srcf: e.tensor_tensor(
                        out=oh[:], in0=srcf[:].unsqueeze(2).to_broadcast([128, 128, 16]),
                        in1=iota16[:].unsqueeze(1).to_broadcast([128, 128, 16]), op=ALU.is_equal),
                        reads=[srcf, iota16], writes=[oh])
                    o("dve", lambda e, pp=pp: e.tensor_tensor(
                        out=oh[:].rearrange("p (h k) i -> p h k i", h=NH), in0=oh[:].rearrange("p (h k) i -> p h k i", h=NH),
                        in1=idx4[:, :, pp, :].unsqueeze(2).to_broadcast([128, NH, 16, 16]), op=ALU.mult),
                        reads=[oh, idxf], writes=[oh])
                    o("dve", lambda e, dst=dst: e.tensor_reduce(out=dst[:], in_=oh[:], op=ALU.add, axis=AX.X), reads=[oh], writes=[dst])
                o("dve", lambda e: e.scalar_tensor_tensor(out=eidf[:], in0=i1s[:], scalar=128.0, in1=i2s[:], op0=ALU.mult, op1=ALU.add),
                  reads=[i1s, i2s], writes=[eidf])
                o("dve", lambda e: e.tensor_copy(out=eid[:], in_=eidf[:]), reads=[eidf], writes=[eid])
                ga, ib = q[n_g0:n_g1], q[n_g1:]
                merged = []
                while ga or ib:
                    if ib:
                        merged.append(ib.pop(0))
                    if ib:
                        merged.append(ib.pop(0))
                    if ga:
                        merged.append(ga.pop(0))
                q[n_g0:] = merged
                return q

            def stageB(i, qn, epi_prev):
                par = i % 2
                ht, xn2, eid, gate = hts[par], xn2s[par], eids[par], gates[par]
                gflat = gate[:].rearrange("p h k -> p (h k)")
                PA = [PS[2], PS[3]] if par == 0 else [PS[6], PS[7]]

                def slot_tail(s):
                    uvg = uvgs[s % NR]
                    dg = dgs[s % 4]
                    c.op("act", lambda e: e.activation(out=gel[:, s:s + 1], in_=hid[:, s:s + 1], func=AF.Gelu), reads=[hid_c[s]], writes=[gel_c[s]])
                    c.op("act", lambda e: e.activation(out=acol[:, s:s + 1], in_=gel[:, s:s + 1], func=AF.Copy, scale=gflat[:, s:s + 1]),
                         reads=[gel_c[s], gate], writes=[acol_c[s]])
                    c.op("act", lambda e: e.activation(out=dg[:], in_=identb[:], func=AF.Copy, scale=acol[:, s:s + 1]),
                         reads=[identb, acol_c[s]], writes=[dg])
                    for hf in range(2):
                        c.op("pe", lambda e, hf=hf: e.matmul(PA[hf][:], lhsT=dg[:], rhs=uvg[:, D + hf * 512:D + (hf + 1) * 512],
                                                             start=(s == 0), stop=(s == 127)),
                             reads=[dg, uvg], writes=[PA[hf]], partial=True)

                LAG = 2
                per = (len(qn) + 119) // 120 if qn else 0
                for s in range(128):
                    uvg = uvgs[s % NR]
                    c.dma("pool", lambda e, uvg=uvg, s=s: e.indirect_dma_start(
                        out=uvg[:], out_offset=None, in_=uv, in_offset=bass.IndirectOffsetOnAxis(ap=eid[:, s:s + 1], axis=0)),
                        reads=[eid, uv_b], writes=[uvg])
                    jb = junkbs[s % 4]
                    c.op("dve", lambda e, uvg=uvg, s=s, jb=jb: e.scalar_tensor_tensor(out=jb[:], in0=xn2[:], scalar=1.0, in1=uvg[:, 0:D],
                                                                                     op0=ALU.mult, op1=ALU.mult, accum_out=hid[:, s:s + 1]),
                         reads=[xn2, uvg], writes=[jb, hid_c[s]])
                    for _ in range(per):
                        if qn:
                            qn.pop(0)()
                    if s == 2:
                        while epi_prev:
                            epi_prev.pop(0)()
                    if s >= LAG:
                        slot_tail(s - LAG)
                for s in range(128 - LAG, 128):
                    slot_tail(s)
                while qn:
                    qn.pop(0)()
                if dbg:
                    c.dma("sp", lambda e: e.dma_start(out=d_hid[i * 128:(i + 1) * 128, :], in_=hid[:]), reads=hid_c, writes=[dbg_b], partial=True)
                    c.dma("sp", lambda e: e.dma_start(out=d_eid[i * 128:(i + 1) * 128, :], in_=eid[:]), reads=[eid], writes=[dbg_b], partial=True)
                    c.dma("sp", lambda e: e.dma_start(out=d_gate[i * 128:(i + 1) * 128, :], in_=gflat), reads=[gate], writes=[dbg_b], partial=True)
                    c.dma("sp", lambda e: e.dma_start(out=d_h[i * 128:(i + 1) * 128, :], in_=ht[:]), reads=[ht], writes=[dbg_b], partial=True)
                epi = []

                def epilogue():
                    for hf in range(2):
                        c.op("dve", lambda e, hf=hf: e.tensor_tensor(out=h2[:, hf * 512:(hf + 1) * 512], in0=PA[hf][:],
                                                                     in1=ht[:, hf * 512:(hf + 1) * 512], op=ALU.add),
                             reads=[PA[hf], ht], writes=[h2], partial=True)
                    c.op("act", lambda e: e.activation(out=junk[:], in_=h2[:], func=AF.Square, accum_out=ssq3[:]), reads=[h2], writes=[junk, ssq3])
                    c.op("act", lambda e: e.activation(out=rstd3[:], in_=ssq3[:], func=AF.Sqrt, scale=1.0 / D, bias=eps[:]),
                         reads=[ssq3, eps], writes=[rstd3])
                    c.op("dve", lambda e: e.reciprocal(out=rstd3[:], in_=rstd3[:]), reads=[rstd3], writes=[rstd3])
                    yt = yo[i % 2]
                    c.op("dve", lambda e: e.scalar_tensor_tensor(out=yt[:], in0=h2[:], scalar=rstd3[:], in1=gfbc[:], op0=ALU.mult, op1=ALU.mult),
                         reads=[h2, rstd3, gfbc], writes=[yt])
                    c.dma("sp", lambda e: e.dma_start(out=y[i * 128:(i + 1) * 128, :], in_=yt[:]), reads=[yt], writes=[y_b[i]])

                epi.append(epilogue)
                return epi

            for th in stageA(0):
                th()
            epi_prev = []
            for i in range(NT):
                qn = stageA(i + 1) if i + 1 < NT else []
                epi_prev = stageB(i, qn, epi_prev)
            while epi_prev:
                epi_prev.pop(0)()
        allb = list(y_b) + [dbg_b]
        if dbg:
            allb += [b for row in qk_b for b in row] + [b for row in va_b for b in row] + [b for row in mx_b for b in row]
        c.finish(allb, "sp")
        c.emit()
    return nc


def _in_map(xb, p, consts):
    m = {"x": np.ascontiguousarray(xb)}
    m.update(p)
    m.update(consts)
    return m


def _prep_params(norm1_g, w_in, b_forget, w_out, norm2_g, peer_wq, peer_subkeys, peer_u, peer_v, normf_g):
    f = lambda a: np.ascontiguousarray(np.asarray(a, dtype=np.float32))
    return {
        "g1T": f(np.asarray(norm1_g)[0].reshape(8, 128).T),
        "w_in": f(np.asarray(w_in)[0]),
        "b_forget": f(np.asarray(b_forget)[0].reshape(1, NH)),
        "w_out": f(np.asarray(w_out)[0]),
        "g2T": f(np.asarray(norm2_g)[0].reshape(8, 128).T),
        "g2": f(np.asarray(norm2_g)[0].reshape(1, D)),
        "peer_wq": f(np.asarray(peer_wq)[0]),
        "peer_subkeys": f(np.asarray(peer_subkeys)[0].reshape(16, 128, 128)),
        "peer_u": f(np.asarray(peer_u)[0]),
        "peer_v": f(np.asarray(peer_v)[0]),
        "normf_g": f(np.asarray(normf_g).reshape(1, D)),
    }


_NC_CACHE = {}


def kernel(x, norm1_g, w_in, b_forget, w_out, norm2_g, peer_wq, peer_subkeys, peer_u, peer_v, normf_g):
    x = np.asarray(x, dtype=np.float32)
    B, T, _ = x.shape
    p = _prep_params(norm1_g, w_in, b_forget, w_out, norm2_g, peer_wq, peer_subkeys, peer_u, peer_v, normf_g)
    consts = _consts()
    if T not in _NC_CACHE:
        _NC_CACHE[T] = build(T)
    nc = _NC_CACHE[T]
    in_maps = [_in_map(x[b], p, consts) for b in range(B)]
    res = run_bass_kernel_spmd(nc, in_maps, core_ids=list(range(B)))
    return np.stack([np.asarray(r["y"], dtype=np.float32) for r in res.results], axis=0)
```

```python
import numpy as np
import ml_dtypes
from contextlib import ExitStack
import concourse.bass as bass
import concourse.mybir as mybir
from concourse.bass_utils import run_bass_kernel_spmd

F32 = mybir.dt.float32
BF16 = mybir.dt.bfloat16
I32 = mybir.dt.int32
U32 = mybir.dt.uint32
AF = mybir.ActivationFunctionType
ALU = mybir.AluOpType
AX = mybir.AxisListType

LIM = 20000
NDMA = 40
NPOOL = 24
ENGS = ("pe", "act", "dve", "pool", "sp")


class Buf:
    def __init__(self, t=None, name=""):
        self.t = t
        self.name = name
        self.w = {}
        self.r = {}

    def __getitem__(self, k):
        return self.t[k]


class Ctx:
    def __init__(self, nc, es):
        self.nc = nc
        self.es = es
        self.root_es = es
        self.ops = {e: [] for e in ENGS}
        self.cnt = {e: 0 for e in ENGS}
        self.esems = {e: [] for e in ENGS}
        self.waited = {e: {} for e in ENGS}
        self.dsems = [es.enter_context(nc.semaphore("dq%d" % i)) for i in range(NDMA + NPOOL)]
        self.dvals = [0] * (NDMA + NPOOL)
        self.drr = 0
        self.prr = 0
        self.nsb = 0
        self.psems = {}
        self.pgen = {}

    def sb(self, shape, dtype, name=None):
        self.nsb += 1
        name = name or "sb%d" % self.nsb
        t = self.es.enter_context(self.nc.sbuf_tensor(name, list(shape), dtype))
        return Buf(t, name)

    def ps(self, shape, dtype, name=None):
        self.nsb += 1
        name = name or "ps%d" % self.nsb
        t = self.es.enter_context(self.nc.psum_tensor(name, list(shape), dtype))
        return Buf(t, name)

    def _esem(self, eng, ep):
        while len(self.esems[eng]) <= ep:
            self.esems[eng].append(
                self.root_es.enter_context(self.nc.semaphore("e_%s_%d" % (eng, len(self.esems[eng]))))
            )
        return self.esems[eng][ep]

    def _wait(self, eng, tok):
        key = tok[:2]
        v = tok[2]
        if self.waited[eng].get(key, 0) >= v:
            return
        self.waited[eng][key] = v
        if tok[0] == "e":
            ep, vv = divmod(v - 1, LIM)
            sem = self._esem(tok[1], ep)
            val = vv + 1
        elif tok[0] == "p":
            sem = self.psems[tok[1]]
            val = 16
        else:
            sem = self.dsems[tok[1]]
            val = v
        self.ops[eng].append(lambda e, s=sem, val=val: e.wait_ge(s, val))

    def _deps(self, eng, reads, writes, partial):
        toks = []
        for b in reads:
            toks.extend(t for (t, _p) in b.w.values())
        for b in writes:
            for (t, p_) in b.w.values():
                if partial and p_:
                    continue
                toks.append(t)
            toks.extend(b.r.values())
        for t in toks:
            if t[0] == "e" and t[1] == eng and eng == "pe":
                continue
            self._wait(eng, t)

    def op(self, eng, fn, reads=(), writes=(), partial=False, pwrites=()):
        self._deps(eng, reads, writes, partial)
        if pwrites:
            self._deps(eng, (), pwrites, True)
        self.cnt[eng] += 1
        n = self.cnt[eng]
        ep, vv = divmod(n - 1, LIM)
        sem = self._esem(eng, ep)
        self.ops[eng].append(lambda e, fn=fn, sem=sem: fn(e).then_inc(sem, 1))
        tok = ("e", eng, n)
        for b in pwrites:
            b.w[tok[:2]] = (tok, True)
        for b in writes:
            if partial:
                b.w[tok[:2]] = (tok, True)
            else:
                b.w = {tok[:2]: (tok, False)}
                b.r = {}
        for b in reads:
            b.r[tok[:2]] = tok
        return tok

    def dma(self, q, fn, reads=(), writes=(), partial=False):
        self._deps(q, reads, writes, partial)
        if q == "pool":
            k = NDMA + self.prr
            self.prr = (self.prr + 1) % NPOOL
        else:
            k = self.drr
            self.drr = (self.drr + 1) % NDMA
        if self.dvals[k] > 0:
            self._wait(q, ("d", k, self.dvals[k]))
        self.dvals[k] += 16
        v = self.dvals[k]
        sem = self.dsems[k]
        self.ops[q].append(lambda e, fn=fn, sem=sem: fn(e).then_inc(sem, 16))
        tok = ("d", k, v)
        for b in writes:
            if partial:
                b.w[tok[:2]] = (tok, True)
            else:
                b.w = {tok[:2]: (tok, False)}
                b.r = {}
        for b in reads:
            b.r[tok[:2]] = tok
        return tok

    def dma_pool(self, fn, dst, reads=()):
        return self.dma("pool", fn, reads=reads, writes=[dst])

    def finish(self, bufs, eng="sp"):
        for b in bufs:
            for (t, _p) in b.w.values():
                self._wait(eng, t)

    def barrier(self):
        for e in ENGS:
            for x in ENGS:
                if x != e and self.cnt[x] > 0:
                    self._wait(e, ("e", x, self.cnt[x]))
            for k in range(NDMA + NPOOL):
                if self.dvals[k] > 0:
                    self._wait(e, ("d", k, self.dvals[k]))

    def phase(self):
        return _Phase(self)

    def emit(self):
        nc = self.nc
        ops = self.ops
        self.ops = {e: [] for e in ENGS}
        self._emit(ops)

    def _emit(self, ops_):
        nc = self.nc
        self.ops_emit = ops_
        with nc.Block() as block:
            @block.tensor
            def _(e):
                for f in self.ops_emit["pe"]:
                    f(e)

            @block.scalar
            def _(e):
                for f in self.ops_emit["act"]:
                    f(e)

            @block.vector
            def _(e):
                for f in self.ops_emit["dve"]:
                    f(e)

            @block.gpsimd
            def _(e):
                for f in self.ops_emit["pool"]:
                    f(e)

            @block.sync
            def _(e):
                for f in self.ops_emit["sp"]:
                    f(e)


class _Phase:
    def __init__(self, c):
        self.c = c

    def __enter__(self):
        self.prev = self.c.es
        self.st = ExitStack()
        self.st.__enter__()
        self.c.es = self.st
        return self

    def __exit__(self, *a):
        if a[0] is None:
            self.c.barrier()
            self.c.emit()
        self.c.es = self.prev
        return self.st.__exit__(*a)

D = 1024
NH = 8
HD = 64
WIN = 3080
MASKW = 2944
NEXP = 16384


def _dil_mask_table():
    kl = np.arange(128)[:, None]
    ci = np.arange(MASKW)[None, :]
    delta = ci - 384 - kl
    ok = delta >= 0
    mult = (ok & (delta <= 128)).astype(np.float64)
    mult = mult + (ok & (delta <= 512) & (delta % 4 == 0))
    mult = mult + (ok & (delta <= 2048) & (delta % 16 == 0))
    slopes = 2.0 ** (-8.0 * np.arange(1, NH + 1) / NH)
    F = np.zeros((128, NH, MASKW), dtype=np.float64)
    for h in range(NH):
        F[:, h, :] = mult * np.exp(-slopes[h] * np.maximum(delta, 0))
    return np.ascontiguousarray(F.reshape(128, NH * MASKW).astype(ml_dtypes.bfloat16))


def _consts():
    c = {}
    c["c_identb"] = np.eye(128, dtype=np.float32).astype(ml_dtypes.bfloat16)
    c["c_identf"] = np.eye(128, dtype=np.float32)
    k = np.arange(128)
    c["c_tri"] = (k[:, None] <= k[None, :]).astype(np.float32)
    c["c_trib"] = (k[None, :] >= k[:, None]).astype(np.float32).astype(ml_dtypes.bfloat16)
    c["c_ones"] = np.ones((128, 128), dtype=np.float32)
    c["c_iota16"] = np.tile(np.arange(16, dtype=np.float32)[None, :], (128, 1))
    c["c_dmask"] = _dil_mask_table()
    return c


def build(T, dbg=False):
    consts_np = _consts()
    NT = T // 128
    NG = T // 512
    nc = bass.Bass("TRN2", target_bir_lowering=False)

    def din(name, shape, dt=F32):
        return nc.dram_tensor(name, list(shape), dt, kind="ExternalInput").ap()

    x = din("x", [T, D])
    g1T = din("g1T", [128, 8])
    w_in = din("w_in", [D, WIN])
    bfg = din("b_forget", [1, NH])
    w_out = din("w_out", [D, D])
    g2T = din("g2T", [128, 8])
    g2 = din("g2", [1, D])
    wq = din("peer_wq", [D, 2048])
    subk = din("peer_subkeys", [16, 128, 128])
    pu = din("peer_u", [NEXP, D])
    pv = din("peer_v", [NEXP, D])
    gf = din("normf_g", [1, D])
    c_identb = din("c_identb", [128, 128], BF16)
    c_identf = din("c_identf", [128, 128])
    c_tri = din("c_tri", [128, 128])
    c_trib = din("c_trib", [128, 128], BF16)
    c_ones = din("c_ones", [128, 128])
    c_iota16 = din("c_iota16", [128, 16])
    c_dmask = din("c_dmask", [128, NH * MASKW], BF16)
    y = nc.dram_tensor("y", [T, D], F32, kind="ExternalOutput").ap()

    skind = "ExternalOutput" if dbg else "Internal"
    qkT = nc.dram_tensor("s_qkT", [2048, T], BF16, kind=skind).ap()
    vaug = nc.dram_tensor("s_vaug", [2, T, NH * 128], BF16, kind=skind).ap()
    mixT = nc.dram_tensor("s_mixT", [D, T], BF16, kind=skind).ap()
    uv = nc.dram_tensor("s_uv", [NEXP, 2 * D], BF16, kind="Internal").ap()
    uv_b = Buf(None)
    if dbg:
        d_L = nc.dram_tensor("d_L", [128, NT * NH], F32, kind="ExternalOutput").ap()
        d_hid = nc.dram_tensor("d_hid", [T, 128], F32, kind="ExternalOutput").ap()
        d_eid = nc.dram_tensor("d_eid", [T, 128], I32, kind="ExternalOutput").ap()
        d_gate = nc.dram_tensor("d_gate", [T, 128], F32, kind="ExternalOutput").ap()
        d_h = nc.dram_tensor("d_h", [T, D], F32, kind="ExternalOutput").ap()
    qk_b = [[Buf(None) for _ in range(NG)] for _ in range(16)]
    va_b = [[Buf(None) for _ in range(NT)] for _ in range(2)]
    mx_b = [[Buf(None) for _ in range(NG)] for _ in range(16)]
    y_b = [Buf(None) for _ in range(NT)]
    dbg_b = Buf(None)

    with ExitStack() as es:
        c = Ctx(nc, es)
        identb = c.sb([128, 128], BF16)
        identf = c.sb([128, 128], F32)
        tri = c.sb([128, 128], F32)
        trib = c.sb([128, 128], BF16)
        ones = c.sb([128, 128], F32)
        iota16 = c.sb([128, 16], F32)
        g1t = c.sb([128, 8], F32)
        g2t = c.sb([128, 8], F32)
        g2bc = c.sb([128, D], F32)
        gfbc = c.sb([128, D], F32)
        bfbc = c.sb([128, NH], F32)
        eps = c.sb([128, 1], F32)
        for t_, src in ((identb, c_identb), (identf, c_identf), (tri, c_tri), (trib, c_trib),
                        (ones, c_ones), (iota16, c_iota16), (g1t, g1T), (g2t, g2T)):
            c.dma("sp", lambda e, t_=t_, src=src: e.dma_start(out=t_[:], in_=src), writes=[t_])
        c.dma("sp", lambda e: e.dma_start(out=g2bc[:], in_=g2.partition_broadcast(128)), writes=[g2bc])
        c.dma("sp", lambda e: e.dma_start(out=gfbc[:], in_=gf.partition_broadcast(128)), writes=[gfbc])
        c.dma("sp", lambda e: e.dma_start(out=bfbc[:], in_=bfg.partition_broadcast(128)), writes=[bfbc])
        c.op("pool", lambda e: e.memset(eps[:], 1e-6), writes=[eps])

        PS = [c.ps([128, 512], F32, name="bank%d" % i) for i in range(8)]

        xts = [c.sb([128, D], F32, name="xt%d" % i) for i in range(2)]
        junk = c.sb([128, D], F32, name="junk")
        xss = [c.sb([128, D], BF16, name="xs%d" % i) for i in range(2)]
        ssq = c.sb([128, 1], F32)
        rstd = c.sb([128, 1], F32)
        Lc = c.sb([128, NT, NH], F32, name="Lc")
        carry = c.sb([128, NT, NH], F32, name="carry")
        tot = c.sb([128, NT, NH], F32, name="tot")
        biasq = c.sb([128, NG, NT, NH], F32, name="biasq")
        with c.phase():
            winb = c.sb([128, 8, WIN], BF16, name="winb")
            wstage = [c.sb([128, WIN], F32, name="wst%d" % i) for i in range(2)]
            for cc in range(8):
                st = wstage[cc % 2]
                c.dma("sp", lambda e, st=st, cc=cc: e.dma_start(out=st[:], in_=w_in[cc * 128:(cc + 1) * 128, :]), writes=[st])
                c.op("dve", lambda e, st=st, cc=cc: e.tensor_scalar(out=winb[:, cc, :], in0=st[:], scalar1=g1t[:, cc:cc + 1],
                                                                     scalar2=None, op0=ALU.mult),
                     reads=[st, g1t], writes=[winb], partial=True)

            xnT = c.sb([128, 8, 512], BF16, name="xnT")
            lf_all = c.sb([128, NT, NH], F32, name="lf_all")
            vst = [c.sb([128, 2, NH, 128], BF16, name="vst%d" % i) for i in range(2)]
            for v_ in vst:
                c.op("pool", lambda e, v_=v_: e.memset(v_[:], 1.0), writes=[v_])
            qst = [c.sb([128, 512], BF16, name="qst%d" % i) for i in range(3)]
            zt = c.sb([128, NH], F32)
            et = c.sb([128, NH], F32)
            qcols = [0, 512, 1544, 2056]
            vcols = [1024, 2568]
            nq = 0
            for g in range(NG):
                for j in range(4):
                    i = g * 4 + j
                    xt = xts[i % 2]
                    xs = xss[i % 2]
                    c.dma("sp", lambda e, xt=xt, i=i: e.dma_start(out=xt[:], in_=x[i * 128:(i + 1) * 128, :]), writes=[xt])
                    c.op("act", lambda e, xt=xt: e.activation(out=junk[:], in_=xt[:], func=AF.Square, accum_out=ssq[:]),
                         reads=[xt], writes=[junk, ssq])
                    c.op("act", lambda e: e.activation(out=rstd[:], in_=ssq[:], func=AF.Sqrt, scale=1.0 / D, bias=eps[:]),
                         reads=[ssq, eps], writes=[rstd])
                    c.op("dve", lambda e: e.reciprocal(out=rstd[:], in_=rstd[:]), reads=[rstd], writes=[rstd])
                    c.op("act", lambda e, xt=xt, xs=xs: e.activation(out=xs[:], in_=xt[:], func=AF.Copy, scale=rstd[:]),
                         reads=[xt, rstd], writes=[xs])
                    pT = PS[0]
                    pTv = pT.t[:].bitcast(BF16)
                    for k in range(8):
                        c.op("pe", lambda e, k=k, xs=xs, pTv=pTv: e.transpose(out=pTv[:, k * 128:(k + 1) * 128],
                                                                              in_=xs[:, k * 128:(k + 1) * 128], identity=identb[:]),
                             reads=[xs, identb], writes=[pT], partial=True)
                    c.op("dve", lambda e, j=j, pTv=pTv: e.tensor_copy(out=xnT[:, :, j * 128:(j + 1) * 128],
                                                                       in_=pTv.rearrange("p (k t) -> p k t", k=8)),
                         reads=[pT], writes=[xnT], partial=True)
                    vs = vst[i % 2]
                    for m in range(2):
                        pv_ = PS[1 + m]
                        for k in range(8):
                            c.op("pe", lambda e, k=k, j=j, m=m, pv_=pv_: e.matmul(pv_[:], lhsT=xnT[:, k, j * 128:(j + 1) * 128],
                                                                                  rhs=winb[:, k, vcols[m]:vcols[m] + 512],
                                                                                  start=(k == 0), stop=(k == 7)),
                                 reads=[xnT, winb], writes=[pv_], partial=True)
                        c.op("act", lambda e, m=m, vs=vs, pv_=pv_: e.copy(out=vs[:, m, :, 0:64],
                                                                           in_=pv_[:].rearrange("p (h d) -> p h d", h=NH)),
                             reads=[pv_], writes=[vs], partial=True)
                    pf = PS[3]
                    for k in range(8):
                        c.op("pe", lambda e, k=k, j=j: e.matmul(pf[:, 0:NH], lhsT=xnT[:, k, j * 128:(j + 1) * 128],
                                                                rhs=winb[:, k, 1536:1536 + NH], start=(k == 0), stop=(k == 7)),
                             reads=[xnT, winb], writes=[pf], partial=True)
                    c.op("dve", lambda e, i=i: e.tensor_tensor(out=lf_all[:, i, :], in0=pf[:, 0:NH], in1=bfbc[:], op=ALU.add),
                         reads=[pf, bfbc], writes=[lf_all], partial=True)
                    for m in range(2):
                        c.dma("pool", lambda e, m=m, vs=vs, i=i: e.dma_start(
                            out=vaug[m, i * 128:(i + 1) * 128, :], in_=vs[:, m, :, :].rearrange("p h d -> p (h d)")),
                            reads=[vs], writes=[va_b[m][i]])
                for f in range(16):
                    cb = qcols[f // 4] + (f % 4) * 128
                    pq = PS[4 + (f % 2)]
                    for k in range(8):
                        c.op("pe", lambda e, k=k, cb=cb, pq=pq: e.matmul(pq[:], lhsT=winb[:, k, cb:cb + 128], rhs=xnT[:, k, :],
                                                                         start=(k == 0), stop=(k == 7)),
                             reads=[winb, xnT], writes=[pq], partial=True)
                    qs = qst[nq % 3]
                    nq += 1
                    eng = "act" if f % 2 == 0 else "dve"
                    if eng == "act":
                        c.op("act", lambda e, qs=qs, pq=pq: e.copy(out=qs[:], in_=pq[:]), reads=[pq], writes=[qs])
                    else:
                        c.op("dve", lambda e, qs=qs, pq=pq: e.tensor_copy(out=qs[:], in_=pq[:]), reads=[pq], writes=[qs])
                    c.dma("pool", lambda e, qs=qs, f=f, g=g: e.dma_start(out=qkT[f * 128:(f + 1) * 128, g * 512:(g + 1) * 512], in_=qs[:]),
                          reads=[qs], writes=[qk_b[f][g]])

            c.op("act", lambda e: e.activation(out=lf_all[:], in_=lf_all[:], func=AF.Exp, scale=-1.0), reads=[lf_all], writes=[lf_all])
            c.op("act", lambda e: e.activation(out=lf_all[:], in_=lf_all[:], func=AF.Ln, bias=1.0), reads=[lf_all], writes=[lf_all])
            pc = PS[6]
            pt_ = PS[7]
            NTH = NT * NH
            lf2 = lf_all[:].rearrange("p t h -> p (t h)")
            c.op("pe", lambda e: e.matmul(pc[:, 0:NTH], lhsT=tri[:], rhs=lf2, start=True, stop=True), reads=[tri, lf_all], writes=[pc])
            c.op("pe", lambda e: e.matmul(pt_[:, 0:NTH], lhsT=ones[:], rhs=lf2, start=True, stop=True), reads=[ones, lf_all], writes=[pt_])
            c.op("dve", lambda e: e.tensor_copy(out=tot[:].rearrange("p t h -> p (t h)"), in_=pt_[:, 0:NTH]), reads=[pt_], writes=[tot])
            c.op("dve", lambda e: e.memset(carry[:, 0, :], 0.0), writes=[carry])
            for i in range(1, NT):
                c.op("dve", lambda e, i=i: e.tensor_tensor(out=carry[:, i, :], in0=carry[:, i - 1, :], in1=tot[:, i - 1, :], op=ALU.add),
                     reads=[carry, tot], writes=[carry])
            c.op("dve", lambda e: e.tensor_tensor(out=Lc[:].rearrange("p t h -> p (t h)"), in0=pc[:, 0:NTH],
                                                  in1=carry[:].rearrange("p t h -> p (t h)"), op=ALU.add),
                 reads=[pc, carry], writes=[Lc])
            for qb in range(NG):
                c.op("dve", lambda e, qb=qb: e.tensor_tensor(
                    out=biasq[:, qb, :, :], in0=Lc[:],
                    in1=carry[:, 4 * qb:4 * qb + 1, :].to_broadcast([128, NT, NH]), op=ALU.subtract),
                    reads=[Lc, carry], writes=[biasq], partial=True)
            if dbg:
                c.dma("sp", lambda e: e.dma_start(out=d_L, in_=Lc[:].rearrange("p t h -> p (t h)")), reads=[Lc], writes=[dbg_b], partial=True)

        with c.phase():
            cast_q = []
            for tbl, c0_ in ((pu, 0), (pv, D)):
                for r0 in range(0, NEXP, 1024):
                    cast_q.append(lambda tbl=tbl, c0_=c0_, r0=r0: c.dma(
                        "pool", lambda e: e.dma_start(out=uv[r0:r0 + 1024, c0_:c0_ + D], in_=tbl[r0:r0 + 1024, :]),
                        writes=[uv_b], partial=True))

            Vaug = c.sb([128, NT, NH, 128], BF16, name="Vaug")
            dmask = c.sb([128, NH, MASKW], BF16, name="dmask")
            c.dma("sp", lambda e: e.dma_start(out=dmask[:].rearrange("p h w -> p (h w)"), in_=c_dmask), writes=[dmask])
            while cast_q:
                cast_q.pop(0)()
            QT = [c.sb([128, T], BF16, name="QT%d" % i) for i in range(2)]
            KT = [c.sb([128, T], BF16, name="KT%d" % i) for i in range(2)]
            c.op("dve", lambda e: e.memset(QT[0][64:128, :], 0.0), writes=[QT[0]])
            c.op("dve", lambda e: e.memset(QT[1][0:64, :], 0.0), writes=[QT[1]])
            NPT = 9
            ptb = [c.sb([128, 512], BF16, name="ptb%d" % i) for i in range(NPT)]
            rz = c.sb([128, 512], F32, name="rz")
            mxs = [c.sb([64, 512], BF16, name="mxs%d" % i) for i in range(2)]
            SB_ = [PS[0], PS[1], PS[4], PS[5], PS[6], PS[7]]
            OB_ = [PS[2], PS[3]]
            dm_np = consts_np["c_dmask"].astype(np.float32).reshape(128, NH, MASKW)

            jobs = [(m, h) for m in range(2) for h in range(NH)]
            pairs = []
            ngrp = 0
            for (m, h) in jobs:
                for qb in range(NG):
                    kb0 = 0 if m == 0 else max(0, 4 * qb - 16)
                    kbl = 4 * qb + 3
                    grp = []
                    for kb in range(kb0, kbl + 1):
                        Dd = 4 * qb - kb
                        c0 = max(0, -128 * Dd)
                        c1 = 512 if m == 0 else min(512, 2176 - 128 * Dd)
                        ci0 = 128 * Dd + c0 + 384
                        if m == 1 and not np.any(dm_np[:, h, ci0:ci0 + (c1 - c0)]):
                            continue
                        grp.append(dict(m=m, h=h, qb=qb, kb=kb, Dd=Dd, c0=c0, c1=c1, ci0=ci0, g=ngrp))
                    grp[0]["first"] = True
                    grp[-1]["last"] = True
                    pairs.extend(grp)
                    ngrp += 1

            def load_job(j):
                m, h = jobs[j]
                if h == 0:
                    for i in range(NT):
                        c.dma("sp", lambda e, m=m, i=i: e.dma_start(out=Vaug[:, i, :, :].rearrange("p h d -> p (h d)"),
                                                                     in_=vaug[m, i * 128:(i + 1) * 128, :]),
                              reads=[va_b[m][i]], writes=[Vaug], partial=True)
                qt_, kt_ = QT[j % 2], KT[j % 2]
                pb = (h % 2) * 64
                qrow = m * 1024 + h * 64
                krow = m * 1024 + 512 + (h // 2) * 128
                qdeps = [qk_b[qrow // 128][g] for g in range(NG)]
                kdeps = [qk_b[krow // 128][g] for g in range(NG)]
                c.dma("sp", lambda e, qt_=qt_, qrow=qrow, pb=pb: e.dma_start(out=qt_[pb:pb + 64, :], in_=qkT[qrow:qrow + 64, :]),
                      reads=qdeps, writes=[qt_], partial=True)
                c.dma("sp", lambda e, kt_=kt_, krow=krow: e.dma_start(out=kt_[:], in_=qkT[krow:krow + 128, :]), reads=kdeps, writes=[kt_])

            def emit_S(n):
                p = pairs[n]
                j = p["m"] * NH + p["h"]
                qt_, kt_ = QT[j % 2], KT[j % 2]
                psb = SB_[n % 6]
                kb, qb, c0, c1 = p["kb"], p["qb"], p["c0"], p["c1"]
                c.op("pe", lambda e: e.matmul(psb[:, c0:c1], lhsT=kt_[:, kb * 128:(kb + 1) * 128],
                                              rhs=qt_[:, qb * 512 + c0:qb * 512 + c1], start=True, stop=True),
                     reads=[kt_, qt_], writes=[psb])

            def emit_exp(n):
                p = pairs[n]
                m, h, kb, qb, c0, c1, Dd, ci0 = p["m"], p["h"], p["kb"], p["qb"], p["c0"], p["c1"], p["Dd"], p["ci0"]
                psb = SB_[n % 6]
                pt = ptb[n % NPT]
                if m == 0:
                    c.op("act", lambda e: e.activation(out=pt[:, c0:c1], in_=psb[:, c0:c1], func=AF.Exp, scale=0.125,
                                                       bias=biasq[:, qb, kb, h:h + 1]),
                         reads=[psb, biasq], writes=[pt])
                    if Dd <= 0:
                        c.op("dve", lambda e: e.tensor_tensor(out=pt[:, c0:c0 + 128], in0=pt[:, c0:c0 + 128], in1=trib[:], op=ALU.mult),
                             reads=[pt, trib], writes=[pt])
                else:
                    c.op("act", lambda e: e.activation(out=pt[:, c0:c1], in_=psb[:, c0:c1], func=AF.Exp, scale=0.125),
                         reads=[psb], writes=[pt])
                    c.op("dve", lambda e: e.tensor_tensor(out=pt[:, c0:c1], in0=pt[:, c0:c1], in1=dmask[:, h, ci0:ci0 + (c1 - c0)], op=ALU.mult),
                         reads=[pt, dmask], writes=[pt])

            def emit_pv(n):
                p = pairs[n]
                m, h, kb, qb, c0, c1 = p["m"], p["h"], p["kb"], p["qb"], p["c0"], p["c1"]
                pt = ptb[n % NPT]
                po = OB_[p["g"] % 2]
                c.op("pe", lambda e: e.matmul(po[:, c0:c1], lhsT=Vaug[:, kb, h, :], rhs=pt[:, c0:c1],
                                              start=bool(p.get("first")), stop=bool(p.get("last")), skip_group_check=True),
                     reads=[Vaug, pt], writes=[po], partial=True)
                if p.get("last"):
                    mx = mxs[p["g"] % 2]
                    if m == 1:
                        c.op("act", lambda e: e.activation(out=rz[64:128, :], in_=po[64:128, :], func=AF.Ln), reads=[po], writes=[rz])
                        c.op("act", lambda e: e.activation(out=rz[64:128, :], in_=rz[64:128, :], func=AF.Exp, scale=-1.0), reads=[rz], writes=[rz])
                    else:
                        c.op("dve", lambda e: e.reciprocal(out=rz[64:128, :], in_=po[64:128, :]), reads=[po], writes=[rz])
                    c.op("dve", lambda e: e.tensor_tensor(out=mx[:], in0=po[0:64, :], in1=rz[64:128, :], op=ALU.mult),
                         reads=[po, rz], writes=[mx])
                    mrow = m * 512 + h * 64
                    c.dma("sp", lambda e: e.dma_start(out=mixT[mrow:mrow + 64, qb * 512:(qb + 1) * 512], in_=mx[:]),
                          reads=[mx], writes=[mx_b[m * 8 + h][qb]])

            BS = 3
            for mm in range(2):
                idxs = [n for n in range(len(pairs)) if pairs[n]["m"] == mm]
                batches = [idxs[i_:i_ + BS] for i_ in range(0, len(idxs), BS)]
                NB = len(batches)
                loaded = set()
                for k in range(NB + 2):
                    if k < NB:
                        for n in batches[k]:
                            j = pairs[n]["m"] * NH + pairs[n]["h"]
                            if j not in loaded:
                                load_job(j)
                                loaded.add(j)
                                if (j + 1) < (mm + 1) * NH and (j + 1) not in loaded:
                                    load_job(j + 1)
                                    loaded.add(j + 1)
                            emit_S(n)
                    if 1 <= k <= NB:
                        for n in batches[k - 1]:
                            emit_exp(n)
                    if 2 <= k <= NB + 1:
                        for n in batches[k - 2]:
                            emit_pv(n)

            while cast_q:
                cast_q.pop(0)()

        with c.phase():
            woutb = c.sb([128, 8, D], BF16, name="woutb")
            Ws = c.sb([128, 8, 2048], BF16, name="Ws")
            with c.phase():
                wstage = [c.sb([128, 2048], F32, name="wst4_%d" % i) for i in range(2)]
                for cc in range(8):
                    st = wstage[cc % 2]
                    c.dma("sp", lambda e, st=st, cc=cc: e.dma_start(out=st[:, 0:D], in_=w_out[cc * 128:(cc + 1) * 128, :]), writes=[st])
                    c.op("dve", lambda e, st=st, cc=cc: e.tensor_copy(out=woutb[:, cc, :], in_=st[:, 0:D]), reads=[st], writes=[woutb], partial=True)
                wqT = c.sb([128, 16, D], BF16, name="wqT")
                skT = c.sb([128, 16, 128], BF16, name="skT")
                nb_ = 0
                for cc in range(8):
                    st = wstage[cc % 2]
                    c.dma("sp", lambda e, st=st, cc=cc: e.dma_start(out=st[:, 0:2048], in_=wq[cc * 128:(cc + 1) * 128, :]), writes=[st])
                    for q4 in range(4):
                        pb = PS[nb_ % 2]
                        nb_ += 1
                        for r in range(4):
                            hp = q4 * 4 + r
                            c.op("pe", lambda e, pb=pb, r=r, hp=hp, st=st: e.transpose(out=pb[:, r * 128:(r + 1) * 128],
                                                                                     in_=st[:, hp * 128:(hp + 1) * 128], identity=identf[:]),
                                 reads=[st, identf], writes=[pb], partial=True)
                        c.op("act", lambda e, pb=pb, q4=q4, cc=cc: e.copy(out=wqT[:, q4 * 4:(q4 + 1) * 4, cc * 128:(cc + 1) * 128],
                                                                          in_=pb[:].rearrange("p (r d) -> p r d", r=4)),
                             reads=[pb], writes=[wqT], partial=True)
                skst = c.sb([128, 16, 128], F32, name="skst")
                c.dma("sp", lambda e: e.dma_start(out=skst[:], in_=subk.rearrange("g k j -> k g j")), writes=[skst])
                for q4 in range(4):
                    pb = PS[nb_ % 2]
                    nb_ += 1
                    for r in range(4):
                        hp = q4 * 4 + r
                        c.op("pe", lambda e, pb=pb, r=r, hp=hp: e.transpose(out=pb[:, r * 128:(r + 1) * 128], in_=skst[:, hp, :], identity=identf[:]),
                             reads=[skst, identf], writes=[pb], partial=True)
                    c.op("act", lambda e, pb=pb, q4=q4: e.copy(out=skT[:, q4 * 4:(q4 + 1) * 4, :], in_=pb[:].rearrange("p (r k) -> p r k", r=4)),
                         reads=[pb], writes=[skT], partial=True)
                for cc in range(8):
                    for q4 in range(4):
                        pb = PS[nb_ % 2]
                        nb_ += 1
                        for r in range(4):
                            hp = q4 * 4 + r
                            c.op("pe", lambda e, pb=pb, r=r, hp=hp, cc=cc: e.matmul(pb[:, r * 128:(r + 1) * 128], lhsT=wqT[:, hp, cc * 128:(cc + 1) * 128],
                                                                                    rhs=skT[:, hp, :], start=True, stop=True),
                                 reads=[wqT, skT], writes=[pb], partial=True)
                        c.op("dve", lambda e, pb=pb, q4=q4, cc=cc: e.tensor_scalar(out=Ws[:, cc, q4 * 512:(q4 + 1) * 512], in0=pb[:],
                                                                                   scalar1=g2t[:, cc:cc + 1], scalar2=None, op0=ALU.mult),
                             reads=[pb, g2t], writes=[Ws], partial=True)

            NR = 12
            mts = [c.sb([128, 8, 128], BF16, name="mt%d" % i) for i in range(2)]
            hts = [c.sb([128, D], F32, name="ht%d" % i) for i in range(2)]
            xn2s = [c.sb([128, D], BF16, name="xn2_%d" % i) for i in range(2)]
            xn2T = c.sb([128, 8, 128], BF16, name="xn2T")
            sc = c.sb([128, 2048], F32, name="sc")
            top = c.sb([128, 16, 16], F32, name="top")
            idx = c.sb([128, 16, 16], U32, name="idx")
            idxf = c.sb([128, 16, 16], F32, name="idxf")
            cand = c.sb([128, NH, 256], F32, name="cand")
            ctop = c.sb([128, NH, 16], F32, name="ctop")
            cpos = c.sb([128, NH, 16], U32, name="cpos")
            ci_ = c.sb([128, 128], U32, name="ci_")
            cj_ = c.sb([128, 128], U32, name="cj_")
            cif = c.sb([128, 128], F32, name="cif")
            cjf = c.sb([128, 128], F32, name="cjf")
            oh = c.sb([128, 128, 16], F32, name="oh")
            i1s = c.sb([128, 128], F32, name="i1s")
            i2s = c.sb([128, 128], F32, name="i2s")
            eidf = c.sb([128, 128], F32, name="eidf")
            eids = [c.sb([128, 128], I32, name="eid%d" % i) for i in range(2)]
            gates = [c.sb([128, NH, 16], F32, name="gate%d" % i) for i in range(2)]
            zs = c.sb([128, NH], F32, name="zs")
            hid = c.sb([128, 128], F32, name="hid")
            gel = c.sb([128, 128], F32, name="gel")
            acol = c.sb([128, 128], F32, name="acol")
            uvgs = [c.sb([128, 2 * D], BF16, name="uvg%d" % i) for i in range(NR)]
            dgs = [c.sb([128, 128], BF16, name="dg%d" % i) for i in range(4)]
            junkbs = [c.sb([128, D], BF16, name="junkb%d" % i) for i in range(4)]
            h2 = c.sb([128, D], F32, name="h2")
            yo = [c.sb([128, D], F32, name="yo%d" % i) for i in range(2)]
            ssq2 = c.sb([128, 1], F32)
            rstd2 = c.sb([128, 1], F32)
            ssq3 = c.sb([128, 1], F32)
            rstd3 = c.sb([128, 1], F32)

            hid_c = [Buf(hid.t, "hid_c%d" % i_) for i_ in range(128)]
            gel_c = [Buf(gel.t, "gel_c%d" % i_) for i_ in range(128)]
            acol_c = [Buf(acol.t, "acol_c%d" % i_) for i_ in range(128)]
            sc_g = [Buf(sc.t, "sc_g%d" % i_) for i_ in range(16)]
            top_g = [Buf(top.t, "top_g%d" % i_) for i_ in range(16)]
            idx_g = [Buf(idx.t, "idx_g%d" % i_) for i_ in range(16)]
            cand_g = [Buf(cand.t, "cand_g%d" % i_) for i_ in range(NH)]
            ctop_g = [Buf(ctop.t, "ctop_g%d" % i_) for i_ in range(NH)]
            cpos_g = [Buf(cpos.t, "cpos_g%d" % i_) for i_ in range(NH)]

            def stageA(i):
                q = []

                def o(*a, **k):
                    q.append(lambda: c.op(*a, **k))

                def dm(*a, **k):
                    q.append(lambda: c.dma(*a, **k))

                def top16_multi(specs):
                    for (src_ap, val_ap, idx_ap, src_b, val_b, idx_b) in specs:
                        o("dve", lambda e, src_ap=src_ap, val_ap=val_ap: e.max(out=val_ap[:, 0:8], in_=src_ap), reads=[src_b], writes=[val_b])
                    for (src_ap, val_ap, idx_ap, src_b, val_b, idx_b) in specs:
                        o("dve", lambda e, src_ap=src_ap, val_ap=val_ap, idx_ap=idx_ap: e.max_index(out=idx_ap[:, 0:8], in_max=val_ap[:, 0:8], in_values=src_ap),
                          reads=[src_b, val_b], writes=[idx_b])
                    for (src_ap, val_ap, idx_ap, src_b, val_b, idx_b) in specs:
                        o("dve", lambda e, src_ap=src_ap, val_ap=val_ap: e.match_replace(out=src_ap, in_to_replace=val_ap[:, 0:8], in_values=src_ap, imm_value=-1e30),
                          reads=[val_b], writes=[src_b])
                    for (src_ap, val_ap, idx_ap, src_b, val_b, idx_b) in specs:
                        o("dve", lambda e, src_ap=src_ap, val_ap=val_ap: e.max(out=val_ap[:, 8:16], in_=src_ap), reads=[src_b], writes=[val_b], partial=True)
                    for (src_ap, val_ap, idx_ap, src_b, val_b, idx_b) in specs:
                        o("dve", lambda e, src_ap=src_ap, val_ap=val_ap, idx_ap=idx_ap: e.max_index(out=idx_ap[:, 8:16], in_max=val_ap[:, 8:16], in_values=src_ap),
                          reads=[src_b, val_b], writes=[idx_b], partial=True)

                g = i // 4
                par = i % 2
                mt, xt, ht, xs, xn2, eid, gate = mts[par], xts[par], hts[par], xss[par], xn2s[par], eids[par], gates[par]
                mdeps = [mx_b[hh][g] for hh in range(16)]
                dm("sp", lambda e: e.dma_start(out=mt[:], in_=mixT.rearrange("(c p) t -> p c t", p=128)[:, :, i * 128:(i + 1) * 128]),
                   reads=mdeps, writes=[mt])
                dm("sp", lambda e: e.dma_start(out=xt[:], in_=x[i * 128:(i + 1) * 128, :]), writes=[xt])
                for hf in range(2):
                    ph = PS[hf]
                    for k in range(8):
                        o("pe", lambda e, ph=ph, k=k, hf=hf: e.matmul(ph[:], lhsT=mt[:, k, :], rhs=woutb[:, k, hf * 512:(hf + 1) * 512],
                                                                        start=(k == 0), stop=(k == 7)),
                          reads=[mt, woutb], writes=[ph], partial=True)
                    o("dve", lambda e, ph=ph, hf=hf: e.tensor_tensor(out=ht[:, hf * 512:(hf + 1) * 512], in0=ph[:],
                                                                     in1=xt[:, hf * 512:(hf + 1) * 512], op=ALU.add),
                      reads=[ph, xt], writes=[ht], partial=True)
                o("act", lambda e: e.activation(out=junk[:], in_=ht[:], func=AF.Square, accum_out=ssq2[:]), reads=[ht], writes=[junk, ssq2])
                o("act", lambda e: e.activation(out=rstd2[:], in_=ssq2[:], func=AF.Sqrt, scale=1.0 / D, bias=eps[:]),
                  reads=[ssq2, eps], writes=[rstd2])
                o("dve", lambda e: e.reciprocal(out=rstd2[:], in_=rstd2[:]), reads=[rstd2], writes=[rstd2])
                o("act", lambda e: e.activation(out=xs[:], in_=ht[:], func=AF.Copy, scale=rstd2[:]), reads=[ht, rstd2], writes=[xs])
                o("dve", lambda e: e.scalar_tensor_tensor(out=xn2[:], in0=ht[:], scalar=rstd2[:], in1=g2bc[:], op0=ALU.mult, op1=ALU.mult),
                  reads=[ht, rstd2, g2bc], writes=[xn2])
                pT = PS[0]
                pTv = pT.t[:].bitcast(BF16)
                for k in range(8):
                    o("pe", lambda e, k=k: e.transpose(out=pTv[:, k * 128:(k + 1) * 128], in_=xs[:, k * 128:(k + 1) * 128], identity=identb[:]),
                      reads=[xs, identb], writes=[pT], partial=True)
                o("dve", lambda e: e.tensor_copy(out=xn2T[:], in_=pTv.rearrange("p (k t) -> p k t", k=8)), reads=[pT], writes=[xn2T])
                for q4 in range(4):
                    pb = PS[4 + (q4 % 2)]
                    for k in range(8):
                        o("pe", lambda e, pb=pb, k=k, q4=q4: e.matmul(pb[:], lhsT=xn2T[:, k, :], rhs=Ws[:, k, q4 * 512:(q4 + 1) * 512],
                                                                      start=(k == 0), stop=(k == 7)),
                          reads=[xn2T, Ws], writes=[pb], partial=True)
                    o("act", lambda e, pb=pb, q4=q4: e.copy(out=sc[:, q4 * 512:(q4 + 1) * 512], in_=pb[:]), reads=[pb], writes=sc_g[4 * q4:4 * q4 + 4])
                top16_multi([(sc[:, gq * 128:(gq + 1) * 128], top[:, gq, :], idx[:, gq, :], sc_g[gq], top_g[gq], idx_g[gq]) for gq in range(16)])
                o("dve", lambda e: e.tensor_copy(out=idxf[:], in_=idx[:]), reads=idx_g, writes=[idxf])
                top4 = top[:].rearrange("p (h two) k -> p h two k", two=2)
                o("dve", lambda e: e.tensor_tensor(
                    out=cand[:].rearrange("p h (a b) -> p h a b", a=16),
                    in0=top4[:, :, 0, :].unsqueeze(3).to_broadcast([128, NH, 16, 16]),
                    in1=top4[:, :, 1, :].unsqueeze(2).to_broadcast([128, NH, 16, 16]), op=ALU.add),
                    reads=top_g, writes=cand_g)
                top16_multi([(cand[:, hh, :], ctop[:, hh, :], cpos[:, hh, :], cand_g[hh], ctop_g[hh], cpos_g[hh]) for hh in range(NH)])
                o("dve", lambda e: e.tensor_tensor(out=gate[:], in0=ctop[:], in1=ctop[:, :, 0:1].to_broadcast([128, NH, 16]), op=ALU.subtract),
                  reads=ctop_g, writes=[gate])
                o("act", lambda e: e.activation(out=gate[:], in_=gate[:], func=AF.Exp), reads=[gate], writes=[gate])
                o("dve", lambda e: e.tensor_reduce(out=zs[:], in_=gate[:], op=ALU.add, axis=AX.X), reads=[gate], writes=[zs])
                o("dve", lambda e: e.reciprocal(out=zs[:], in_=zs[:]), reads=[zs], writes=[zs])
                o("dve", lambda e: e.tensor_tensor(out=gate[:], in0=gate[:], in1=zs[:].unsqueeze(2).to_broadcast([128, NH, 16]), op=ALU.mult),
                  reads=[gate, zs], writes=[gate])
                cp2 = cpos[:].rearrange("p h k -> p (h k)")
                o("dve", lambda e: e.tensor_single_scalar(out=ci_[:], in_=cp2, scalar=4, op=ALU.logical_shift_right), reads=cpos_g, writes=[ci_])
                o("dve", lambda e: e.tensor_single_scalar(out=cj_[:], in_=cp2, scalar=15, op=ALU.bitwise_and), reads=cpos_g, writes=[cj_])
                o("dve", lambda e: e.tensor_copy(out=cif[:], in_=ci_[:]), reads=[ci_], writes=[cif])
                o("dve", lambda e: e.tensor_copy(out=cjf[:], in_=cj_[:]), reads=[cj_], writes=[cjf])
                idx4 = idxf[:].rearrange("p (h two) k -> p h two k", two=2)
                for (srcf, pp, dst) in ((cif, 0, i1s), (cjf, 1, i2s)):
                    o("dve", lambda e, srcf=srcf: e.tensor_tensor(
                        out=oh[:], in0=srcf[:].unsqueeze(2).to_broadcast([128, 128, 16]),
                        in1=iota16[:].unsqueeze(1).to_broadcast([128, 128, 16]), op=ALU.is_equal),
                        reads=[srcf, iota16], writes=[oh])
                    o("dve", lambda e, pp=pp: e.tensor_tensor(
                        out=oh[:].rearrange("p (h k) i -> p h k i", h=NH), in0=oh[:].rearrange("p (h k) i -> p h k i", h=NH),
                        in1=idx4[:, :, pp, :].unsqueeze(2).to_broadcast([128, NH, 16, 16]), op=ALU.mult),
                        reads=[oh, idxf], writes=[oh])
                    o("dve", lambda e, dst=dst: e.tensor_reduce(out=dst[:], in_=oh[:], op=ALU.add, axis=AX.X), reads=[oh], writes=[dst])
                o("dve", lambda e: e.scalar_tensor_tensor(out=eidf[:], in0=i1s[:], scalar=128.0, in1=i2s[:], op0=ALU.mult, op1=ALU.add),
                  reads=[i1s, i2s], writes=[eidf])
                o("dve", lambda e: e.tensor_copy(out=eid[:], in_=eidf[:]), reads=[eidf], writes=[eid])
                return q

            def stageB(i, qn, epi_prev):
                par = i % 2
                ht, xn2, eid, gate = hts[par], xn2s[par], eids[par], gates[par]
                gflat = gate[:].rearrange("p h k -> p (h k)")
                PA = [PS[2], PS[3]] if par == 0 else [PS[6], PS[7]]

                def slot_tail(s):
                    uvg = uvgs[s % NR]
                    dg = dgs[s % 4]
                    c.op("act", lambda e: e.activation(out=gel[:, s:s + 1], in_=hid[:, s:s + 1], func=AF.Gelu), reads=[hid_c[s]], writes=[gel_c[s]])
                    c.op("act", lambda e: e.activation(out=acol[:, s:s + 1], in_=gel[:, s:s + 1], func=AF.Copy, scale=gflat[:, s:s + 1]),
                         reads=[gel_c[s], gate], writes=[acol_c[s]])
                    c.op("act", lambda e: e.activation(out=dg[:], in_=identb[:], func=AF.Copy, scale=acol[:, s:s + 1]),
                         reads=[identb, acol_c[s]], writes=[dg])
                    for hf in range(2):
                        c.op("pe", lambda e, hf=hf: e.matmul(PA[hf][:], lhsT=dg[:], rhs=uvg[:, D + hf * 512:D + (hf + 1) * 512],
                                                             start=(s == 0), stop=(s == 127)),
                             reads=[dg, uvg], writes=[PA[hf]], partial=True)

                LAG = 2
                per = (len(qn) + 119) // 120 if qn else 0
                for s in range(128):
                    uvg = uvgs[s % NR]
                    c.dma("pool", lambda e, uvg=uvg, s=s: e.indirect_dma_start(
                        out=uvg[:], out_offset=None, in_=uv, in_offset=bass.IndirectOffsetOnAxis(ap=eid[:, s:s + 1], axis=0)),
                        reads=[eid, uv_b], writes=[uvg])
                    jb = junkbs[s % 4]
                    c.op("dve", lambda e, uvg=uvg, s=s, jb=jb: e.scalar_tensor_tensor(out=jb[:], in0=xn2[:], scalar=1.0, in1=uvg[:, 0:D],
                                                                                     op0=ALU.mult, op1=ALU.mult, accum_out=hid[:, s:s + 1]),
                         reads=[xn2, uvg], writes=[jb, hid_c[s]])
                    for _ in range(per):
                        if qn:
                            qn.pop(0)()
                    if s == 2:
                        while epi_prev:
                            epi_prev.pop(0)()
                    if s >= LAG:
                        slot_tail(s - LAG)
                for s in range(128 - LAG, 128):
                    slot_tail(s)
                while qn:
                    qn.pop(0)()
                if dbg:
                    c.dma("sp", lambda e: e.dma_start(out=d_hid[i * 128:(i + 1) * 128, :], in_=hid[:]), reads=hid_c, writes=[dbg_b], partial=True)
                    c.dma("sp", lambda e: e.dma_start(out=d_eid[i * 128:(i + 1) * 128, :], in_=eid[:]), reads=[eid], writes=[dbg_b], partial=True)
                    c.dma("sp", lambda e: e.dma_start(out=d_gate[i * 128:(i + 1) * 128, :], in_=gflat), reads=[gate], writes=[dbg_b], partial=True)
                    c.dma("sp", lambda e: e.dma_start(out=d_h[i * 128:(i + 1) * 128, :], in_=ht[:]), reads=[ht], writes=[dbg_b], partial=True)
                epi = []

                def epilogue():
                    for hf in range(2):
                        c.op("dve", lambda e, hf=hf: e.tensor_tensor(out=h2[:, hf * 512:(hf + 1) * 512], in0=PA[hf][:],
                                                                     in1=ht[:, hf * 512:(hf + 1) * 512], op=ALU.add),
                             reads=[PA[hf], ht], writes=[h2], partial=True)
                    c.op("act", lambda e: e.activation(out=junk[:], in_=h2[:], func=AF.Square, accum_out=ssq3[:]), reads=[h2], writes=[junk, ssq3])
                    c.op("act", lambda e: e.activation(out=rstd3[:], in_=ssq3[:], func=AF.Sqrt, scale=1.0 / D, bias=eps[:]),
                         reads=[ssq3, eps], writes=[rstd3])
                    c.op("dve", lambda e: e.reciprocal(out=rstd3[:], in_=rstd3[:]), reads=[rstd3], writes=[rstd3])
                    yt = yo[i % 2]
                    c.op("dve", lambda e: e.scalar_tensor_tensor(out=yt[:], in0=h2[:], scalar=rstd3[:], in1=gfbc[:], op0=ALU.mult, op1=ALU.mult),
                         reads=[h2, rstd3, gfbc], writes=[yt])
                    c.dma("sp", lambda e: e.dma_start(out=y[i * 128:(i + 1) * 128, :], in_=yt[:]), reads=[yt], writes=[y_b[i]])

                epi.append(epilogue)
                return epi

            for th in stageA(0):
                th()
            epi_prev = []
            for i in range(NT):
                qn = stageA(i + 1) if i + 1 < NT else []
                epi_prev = stageB(i, qn, epi_prev)
            while epi_prev:
                epi_prev.pop(0)()
        allb = list(y_b) + [dbg_b]
        if dbg:
            allb += [b for row in qk_b for b in row] + [b for row in va_b for b in row] + [b for row in mx_b for b in row]
        c.finish(allb, "sp")
        c.emit()
    return nc


def _in_map(xb, p, consts):
    m = {"x": np.ascontiguousarray(xb)}
    m.update(p)
    m.update(consts)
    return m


def _prep_params(norm1_g, w_in, b_forget, w_out, norm2_g, peer_wq, peer_subkeys, peer_u, peer_v, normf_g):
    f = lambda a: np.ascontiguousarray(np.asarray(a, dtype=np.float32))
    return {
        "g1T": f(np.asarray(norm1_g)[0].reshape(8, 128).T),
        "w_in": f(np.asarray(w_in)[0]),
        "b_forget": f(np.asarray(b_forget)[0].reshape(1, NH)),
        "w_out": f(np.asarray(w_out)[0]),
        "g2T": f(np.asarray(norm2_g)[0].reshape(8, 128).T),
        "g2": f(np.asarray(norm2_g)[0].reshape(1, D)),
        "peer_wq": f(np.asarray(peer_wq)[0]),
        "peer_subkeys": f(np.asarray(peer_subkeys)[0].reshape(16, 128, 128)),
        "peer_u": f(np.asarray(peer_u)[0]),
        "peer_v": f(np.asarray(peer_v)[0]),
        "normf_g": f(np.asarray(normf_g).reshape(1, D)),
    }


_NC_CACHE = {}


def kernel(x, norm1_g, w_in, b_forget, w_out, norm2_g, peer_wq, peer_subkeys, peer_u, peer_v, normf_g):
    x = np.asarray(x, dtype=np.float32)
    B, T, _ = x.shape
    p = _prep_params(norm1_g, w_in, b_forget, w_out, norm2_g, peer_wq, peer_subkeys, peer_u, peer_v, normf_g)
    consts = _consts()
    if T not in _NC_CACHE:
        _NC_CACHE[T] = build(T)
    nc = _NC_CACHE[T]
    in_maps = [_in_map(x[b], p, consts) for b in range(B)]
    res = run_bass_kernel_spmd(nc, in_maps, core_ids=list(range(B)))
    return np.stack([np.asarray(r["y"], dtype=np.float32) for r in res.results], axis=0)
```

```python
import numpy as np
import ml_dtypes
from contextlib import ExitStack
import concourse.bass as bass
import concourse.mybir as mybir
from concourse.bass_utils import run_bass_kernel_spmd

F32 = mybir.dt.float32
BF16 = mybir.dt.bfloat16
I32 = mybir.dt.int32
U32 = mybir.dt.uint32
AF = mybir.ActivationFunctionType
ALU = mybir.AluOpType
AX = mybir.AxisListType

LIM = 20000
NDMA = 40
NPOOL = 24
ENGS = ("pe", "act", "dve", "pool", "sp")


class Buf:
    def __init__(self, t=None, name=""):
        self.t = t
        self.name = name
        self.w = {}
        self.r = {}

    def __getitem__(self, k):
        return self.t[k]


class Ctx:
    def __init__(self, nc, es):
        self.nc = nc
        self.es = es
        self.root_es = es
        self.ops = {e: [] for e in ENGS}
        self.cnt = {e: 0 for e in ENGS}
        self.esems = {e: [] for e in ENGS}
        self.waited = {e: {} for e in ENGS}
        self.dsems = [es.enter_context(nc.semaphore("dq%d" % i)) for i in range(NDMA + NPOOL)]
        self.dvals = [0] * (NDMA + NPOOL)
        self.drr = 0
        self.prr = 0
        self.nsb = 0
        self.psems = {}
        self.pgen = {}

    def sb(self, shape, dtype, name=None):
        self.nsb += 1
        name = name or "sb%d" % self.nsb
        t = self.es.enter_context(self.nc.sbuf_tensor(name, list(shape), dtype))
        return Buf(t, name)

    def ps(self, shape, dtype, name=None):
        self.nsb += 1
        name = name or "ps%d" % self.nsb
        t = self.es.enter_context(self.nc.psum_tensor(name, list(shape), dtype))
        return Buf(t, name)

    def _esem(self, eng, ep):
        while len(self.esems[eng]) <= ep:
            self.esems[eng].append(
                self.root_es.enter_context(self.nc.semaphore("e_%s_%d" % (eng, len(self.esems[eng]))))
            )
        return self.esems[eng][ep]

    def _wait(self, eng, tok):
        key = tok[:2]
        v = tok[2]
        if self.waited[eng].get(key, 0) >= v:
            return
        self.waited[eng][key] = v
        if tok[0] == "e":
            ep, vv = divmod(v - 1, LIM)
            sem = self._esem(tok[1], ep)
            val = vv + 1
        elif tok[0] == "p":
            sem = self.psems[tok[1]]
            val = 16
        else:
            sem = self.dsems[tok[1]]
            val = v
        self.ops[eng].append(lambda e, s=sem, val=val: e.wait_ge(s, val))

    def _deps(self, eng, reads, writes, partial):
        toks = []
        for b in reads:
            toks.extend(t for (t, _p) in b.w.values())
        for b in writes:
            for (t, p_) in b.w.values():
                if partial and p_:
                    continue
                toks.append(t)
            toks.extend(b.r.values())
        for t in toks:
            if t[0] == "e" and t[1] == eng and eng == "pe":
                continue
            self._wait(eng, t)

    def op(self, eng, fn, reads=(), writes=(), partial=False, pwrites=()):
        self._deps(eng, reads, writes, partial)
        if pwrites:
            self._deps(eng, (), pwrites, True)
        self.cnt[eng] += 1
        n = self.cnt[eng]
        ep, vv = divmod(n - 1, LIM)
        sem = self._esem(eng, ep)
        self.ops[eng].append(lambda e, fn=fn, sem=sem: fn(e).then_inc(sem, 1))
        tok = ("e", eng, n)
        for b in pwrites:
            b.w[tok[:2]] = (tok, True)
        for b in writes:
            if partial:
                b.w[tok[:2]] = (tok, True)
            else:
                b.w = {tok[:2]: (tok, False)}
                b.r = {}
        for b in reads:
            b.r[tok[:2]] = tok
        return tok

    def dma(self, q, fn, reads=(), writes=(), partial=False):
        self._deps(q, reads, writes, partial)
        if q == "pool":
            k = NDMA + self.prr
            self.prr = (self.prr + 1) % NPOOL
        else:
            k = self.drr
            self.drr = (self.drr + 1) % NDMA
        if self.dvals[k] > 0:
            self._wait(q, ("d", k, self.dvals[k]))
        self.dvals[k] += 16
        v = self.dvals[k]
        sem = self.dsems[k]
        self.ops[q].append(lambda e, fn=fn, sem=sem: fn(e).then_inc(sem, 16))
        tok = ("d", k, v)
        for b in writes:
            if partial:
                b.w[tok[:2]] = (tok, True)
            else:
                b.w = {tok[:2]: (tok, False)}
                b.r = {}
        for b in reads:
            b.r[tok[:2]] = tok
        return tok

    def dma_pool(self, fn, dst, reads=()):
        return self.dma("pool", fn, reads=reads, writes=[dst])

    def finish(self, bufs, eng="sp"):
        for b in bufs:
            for (t, _p) in b.w.values():
                self._wait(eng, t)

    def barrier(self):
        for e in ENGS:
            for x in ENGS:
                if x != e and self.cnt[x] > 0:
                    self._wait(e, ("e", x, self.cnt[x]))
            for k in range(NDMA + NPOOL):
                if self.dvals[k] > 0:
                    self._wait(e, ("d", k, self.dvals[k]))

    def phase(self):
        return _Phase(self)

    def emit(self):
        nc = self.nc
        ops = self.ops
        self.ops = {e: [] for e in ENGS}
        self._emit(ops)

    def _emit(self, ops_):
        nc = self.nc
        self.ops_emit = ops_
        with nc.Block() as block:
            @block.tensor
            def _(e):
                for f in self.ops_emit["pe"]:
                    f(e)

            @block.scalar
            def _(e):
                for f in self.ops_emit["act"]:
                    f(e)

            @block.vector
            def _(e):
                for f in self.ops_emit["dve"]:
                    f(e)

            @block.gpsimd
            def _(e):
                for f in self.ops_emit["pool"]:
                    f(e)

            @block.sync
            def _(e):
                for f in self.ops_emit["sp"]:
                    f(e)


class _Phase:
    def __init__(self, c):
        self.c = c

    def __enter__(self):
        self.prev = self.c.es
        self.st = ExitStack()
        self.st.__enter__()
        self.c.es = self.st
        return self

    def __exit__(self, *a):
        if a[0] is None:
            self.c.barrier()
            self.c.emit()
        self.c.es = self.prev
        return self.st.__exit__(*a)

D = 1024
NH = 8
HD = 64
WIN = 3080
MASKW = 2944
NEXP = 16384


def _dil_mask_table():
    kl = np.arange(128)[:, None]
    ci = np.arange(MASKW)[None, :]
    delta = ci - 384 - kl
    ok = delta >= 0
    mult = (ok & (delta <= 128)).astype(np.float64)
    mult = mult + (ok & (delta <= 512) & (delta % 4 == 0))
    mult = mult + (ok & (delta <= 2048) & (delta % 16 == 0))
    slopes = 2.0 ** (-8.0 * np.arange(1, NH + 1) / NH)
    F = np.zeros((128, NH, MASKW), dtype=np.float64)
    for h in range(NH):
        F[:, h, :] = mult * np.exp(-slopes[h] * np.maximum(delta, 0))
    return np.ascontiguousarray(F.reshape(128, NH * MASKW).astype(ml_dtypes.bfloat16))


def _consts():
    c = {}
    c["c_identb"] = np.eye(128, dtype=np.float32).astype(ml_dtypes.bfloat16)
    c["c_identf"] = np.eye(128, dtype=np.float32)
    k = np.arange(128)
    c["c_tri"] = (k[:, None] <= k[None, :]).astype(np.float32)
    c["c_trib"] = (k[None, :] >= k[:, None]).astype(np.float32).astype(ml_dtypes.bfloat16)
    c["c_ones"] = np.ones((128, 128), dtype=np.float32)
    c["c_iota16"] = np.tile(np.arange(16, dtype=np.float32)[None, :], (128, 1))
    c["c_dmask"] = _dil_mask_table()
    return c


def build(T, dbg=False):
    consts_np = _consts()
    NT = T // 128
    NG = T // 512
    nc = bass.Bass("TRN2", target_bir_lowering=False)

    def din(name, shape, dt=F32):
        return nc.dram_tensor(name, list(shape), dt, kind="ExternalInput").ap()

    x = din("x", [T, D])
    g1T = din("g1T", [128, 8])
    w_in = din("w_in", [D, WIN])
    bfg = din("b_forget", [1, NH])
    w_out = din("w_out", [D, D])
    g2T = din("g2T", [128, 8])
    g2 = din("g2", [1, D])
    wq = din("peer_wq", [D, 2048])
    subk = din("peer_subkeys", [16, 128, 128])
    pu = din("peer_u", [NEXP, D])
    pv = din("peer_v", [NEXP, D])
    gf = din("normf_g", [1, D])
    c_identb = din("c_identb", [128, 128], BF16)
    c_identf = din("c_identf", [128, 128])
    c_tri = din("c_tri", [128, 128])
    c_trib = din("c_trib", [128, 128], BF16)
    c_ones = din("c_ones", [128, 128])
    c_iota16 = din("c_iota16", [128, 16])
    c_dmask = din("c_dmask", [128, NH * MASKW], BF16)
    y = nc.dram_tensor("y", [T, D], F32, kind="ExternalOutput").ap()

    skind = "ExternalOutput" if dbg else "Internal"
    qkT = nc.dram_tensor("s_qkT", [2048, T], BF16, kind=skind).ap()
    vaug = nc.dram_tensor("s_vaug", [2, T, NH * 128], BF16, kind=skind).ap()
    mixT = nc.dram_tensor("s_mixT", [D, T], BF16, kind=skind).ap()
    uv = nc.dram_tensor("s_uv", [NEXP, 2 * D], BF16, kind="Internal").ap()
    uv_b = Buf(None)
    if dbg:
        d_L = nc.dram_tensor("d_L", [128, NT * NH], F32, kind="ExternalOutput").ap()
        d_hid = nc.dram_tensor("d_hid", [T, 128], F32, kind="ExternalOutput").ap()
        d_eid = nc.dram_tensor("d_eid", [T, 128], I32, kind="ExternalOutput").ap()
        d_gate = nc.dram_tensor("d_gate", [T, 128], F32, kind="ExternalOutput").ap()
        d_h = nc.dram_tensor("d_h", [T, D], F32, kind="ExternalOutput").ap()
    qk_b = [[Buf(None) for _ in range(NG)] for _ in range(16)]
    va_b = [[Buf(None) for _ in range(NT)] for _ in range(2)]
    mx_b = [[Buf(None) for _ in range(NG)] for _ in range(16)]
    y_b = [Buf(None) for _ in range(NT)]
    dbg_b = Buf(None)

    with ExitStack() as es:
        c = Ctx(nc, es)
        identb = c.sb([128, 128], BF16)
        identf = c.sb([128, 128], F32)
        tri = c.sb([128, 128], F32)
        trib = c.sb([128, 128], BF16)
        ones = c.sb([128, 128], F32)
        iota16 = c.sb([128, 16], F32)
        g1t = c.sb([128, 8], F32)
        g2t = c.sb([128, 8], F32)
        g2bc = c.sb([128, D], F32)
        gfbc = c.sb([128, D], F32)
        bfbc = c.sb([128, NH], F32)
        eps = c.sb([128, 1], F32)
        for t_, src in ((identb, c_identb), (identf, c_identf), (tri, c_tri), (trib, c_trib),
                        (ones, c_ones), (iota16, c_iota16), (g1t, g1T), (g2t, g2T)):
            c.dma("sp", lambda e, t_=t_, src=src: e.dma_start(out=t_[:], in_=src), writes=[t_])
        c.dma("sp", lambda e: e.dma_start(out=g2bc[:], in_=g2.partition_broadcast(128)), writes=[g2bc])
        c.dma("sp", lambda e: e.dma_start(out=gfbc[:], in_=gf.partition_broadcast(128)), writes=[gfbc])
        c.dma("sp", lambda e: e.dma_start(out=bfbc[:], in_=bfg.partition_broadcast(128)), writes=[bfbc])
        c.op("pool", lambda e: e.memset(eps[:], 1e-6), writes=[eps])

        PS = [c.ps([128, 512], F32, name="bank%d" % i) for i in range(8)]

        xts = [c.sb([128, D], F32, name="xt%d" % i) for i in range(2)]
        junk = c.sb([128, D], F32, name="junk")
        xss = [c.sb([128, D], BF16, name="xs%d" % i) for i in range(2)]
        ssq = c.sb([128, 1], F32)
        rstd = c.sb([128, 1], F32)
        Lc = c.sb([128, NT, NH], F32, name="Lc")
        carry = c.sb([128, NT, NH], F32, name="carry")
        tot = c.sb([128, NT, NH], F32, name="tot")
        biasq = c.sb([128, NG, NT, NH], F32, name="biasq")
        with c.phase():
            winb = c.sb([128, 8, WIN], BF16, name="winb")
            wstage = [c.sb([128, WIN], F32, name="wst%d" % i) for i in range(2)]
            for cc in range(8):
                st = wstage[cc % 2]
                c.dma("sp", lambda e, st=st, cc=cc: e.dma_start(out=st[:], in_=w_in[cc * 128:(cc + 1) * 128, :]), writes=[st])
                c.op("dve", lambda e, st=st, cc=cc: e.tensor_scalar(out=winb[:, cc, :], in0=st[:], scalar1=g1t[:, cc:cc + 1],
                                                                     scalar2=None, op0=ALU.mult),
                     reads=[st, g1t], writes=[winb], partial=True)

            xnT = c.sb([128, 8, 512], BF16, name="xnT")
            lf_all = c.sb([128, NT, NH], F32, name="lf_all")
            vst = [c.sb([128, 2, NH, 128], BF16, name="vst%d" % i) for i in range(2)]
            for v_ in vst:
                c.op("pool", lambda e, v_=v_: e.memset(v_[:], 1.0), writes=[v_])
            qst = [c.sb([128, 512], BF16, name="qst%d" % i) for i in range(3)]
            zt = c.sb([128, NH], F32)
            et = c.sb([128, NH], F32)
            qcols = [0, 512, 1544, 2056]
            vcols = [1024, 2568]
            nq = 0
            for g in range(NG):
                for j in range(4):
                    i = g * 4 + j
                    xt = xts[i % 2]
                    xs = xss[i % 2]
                    c.dma("sp", lambda e, xt=xt, i=i: e.dma_start(out=xt[:], in_=x[i * 128:(i + 1) * 128, :]), writes=[xt])
                    c.op("dve", lambda e, xt=xt: e.scalar_tensor_tensor(out=junk[:], in0=xt[:], scalar=1.0, in1=xt[:], op0=ALU.mult, op1=ALU.mult,
                                                                        accum_out=ssq[:]),
                         reads=[xt], writes=[junk, ssq])
                    c.op("act", lambda e: e.activation(out=rstd[:], in_=ssq[:], func=AF.Sqrt, scale=1.0 / D, bias=eps[:]),
                         reads=[ssq, eps], writes=[rstd])
                    c.op("dve", lambda e: e.reciprocal(out=rstd[:], in_=rstd[:]), reads=[rstd], writes=[rstd])
                    c.op("act", lambda e, xt=xt, xs=xs: e.activation(out=xs[:], in_=xt[:], func=AF.Copy, scale=rstd[:]),
                         reads=[xt, rstd], writes=[xs])
                    pT = PS[0]
                    pTv = pT.t[:].bitcast(BF16)
                    for k in range(8):
                        c.op("pe", lambda e, k=k, xs=xs, pTv=pTv: e.transpose(out=pTv[:, k * 128:(k + 1) * 128],
                                                                              in_=xs[:, k * 128:(k + 1) * 128], identity=identb[:]),
                             reads=[xs, identb], writes=[pT], partial=True)
                    c.op("dve", lambda e, j=j, pTv=pTv: e.tensor_copy(out=xnT[:, :, j * 128:(j + 1) * 128],
                                                                       in_=pTv.rearrange("p (k t) -> p k t", k=8)),
                         reads=[pT], writes=[xnT], partial=True)
                    vs = vst[i % 2]
                    for m in range(2):
                        pv_ = PS[1 + m]
                        for k in range(8):
                            c.op("pe", lambda e, k=k, j=j, m=m, pv_=pv_: e.matmul(pv_[:], lhsT=xnT[:, k, j * 128:(j + 1) * 128],
                                                                                  rhs=winb[:, k, vcols[m]:vcols[m] + 512],
                                                                                  start=(k == 0), stop=(k == 7)),
                                 reads=[xnT, winb], writes=[pv_], partial=True)
                        c.op("act", lambda e, m=m, vs=vs, pv_=pv_: e.copy(out=vs[:, m, :, 0:64],
                                                                           in_=pv_[:].rearrange("p (h d) -> p h d", h=NH)),
                             reads=[pv_], writes=[vs], partial=True)
                    pf = PS[3]
                    for k in range(8):
                        c.op("pe", lambda e, k=k, j=j: e.matmul(pf[:, 0:NH], lhsT=xnT[:, k, j * 128:(j + 1) * 128],
                                                                rhs=winb[:, k, 1536:1536 + NH], start=(k == 0), stop=(k == 7)),
                             reads=[xnT, winb], writes=[pf], partial=True)
                    c.op("dve", lambda e, i=i: e.tensor_tensor(out=lf_all[:, i, :], in0=pf[:, 0:NH], in1=bfbc[:], op=ALU.add),
                         reads=[pf, bfbc], writes=[lf_all], partial=True)
                    for m in range(2):
                        c.dma("pool", lambda e, m=m, vs=vs, i=i: e.dma_start(
                            out=vaug[m, i * 128:(i + 1) * 128, :], in_=vs[:, m, :, :].rearrange("p h d -> p (h d)")),
                            reads=[vs], writes=[va_b[m][i]])
                for f in range(16):
                    cb = qcols[f // 4] + (f % 4) * 128
                    pq = PS[4 + (f % 2)]
                    for k in range(8):
                        c.op("pe", lambda e, k=k, cb=cb, pq=pq: e.matmul(pq[:], lhsT=winb[:, k, cb:cb + 128], rhs=xnT[:, k, :],
                                                                         start=(k == 0), stop=(k == 7)),
                             reads=[winb, xnT], writes=[pq], partial=True)
                    qs = qst[nq % 3]
                    nq += 1
                    eng = "act" if f % 2 == 0 else "dve"
                    if eng == "act":
                        c.op("act", lambda e, qs=qs, pq=pq: e.copy(out=qs[:], in_=pq[:]), reads=[pq], writes=[qs])
                    else:
                        c.op("dve", lambda e, qs=qs, pq=pq: e.tensor_copy(out=qs[:], in_=pq[:]), reads=[pq], writes=[qs])
                    c.dma("pool", lambda e, qs=qs, f=f, g=g: e.dma_start(out=qkT[f * 128:(f + 1) * 128, g * 512:(g + 1) * 512], in_=qs[:]),
                          reads=[qs], writes=[qk_b[f][g]])

            c.op("act", lambda e: e.activation(out=lf_all[:], in_=lf_all[:], func=AF.Exp, scale=-1.0), reads=[lf_all], writes=[lf_all])
            c.op("act", lambda e: e.activation(out=lf_all[:], in_=lf_all[:], func=AF.Ln, bias=1.0), reads=[lf_all], writes=[lf_all])
            pc = PS[6]
            pt_ = PS[7]
            NTH = NT * NH
            lf2 = lf_all[:].rearrange("p t h -> p (t h)")
            c.op("pe", lambda e: e.matmul(pc[:, 0:NTH], lhsT=tri[:], rhs=lf2, start=True, stop=True), reads=[tri, lf_all], writes=[pc])
            c.op("pe", lambda e: e.matmul(pt_[:, 0:NTH], lhsT=ones[:], rhs=lf2, start=True, stop=True), reads=[ones, lf_all], writes=[pt_])
            c.op("dve", lambda e: e.tensor_copy(out=tot[:].rearrange("p t h -> p (t h)"), in_=pt_[:, 0:NTH]), reads=[pt_], writes=[tot])
            c.op("dve", lambda e: e.memset(carry[:, 0, :], 0.0), writes=[carry])
            for i in range(1, NT):
                c.op("dve", lambda e, i=i: e.tensor_tensor(out=carry[:, i, :], in0=carry[:, i - 1, :], in1=tot[:, i - 1, :], op=ALU.add),
                     reads=[carry, tot], writes=[carry])
            c.op("dve", lambda e: e.tensor_tensor(out=Lc[:].rearrange("p t h -> p (t h)"), in0=pc[:, 0:NTH],
                                                  in1=carry[:].rearrange("p t h -> p (t h)"), op=ALU.add),
                 reads=[pc, carry], writes=[Lc])
            for qb in range(NG):
                c.op("dve", lambda e, qb=qb: e.tensor_tensor(
                    out=biasq[:, qb, :, :], in0=Lc[:],
                    in1=carry[:, 4 * qb:4 * qb + 1, :].to_broadcast([128, NT, NH]), op=ALU.subtract),
                    reads=[Lc, carry], writes=[biasq], partial=True)
            if dbg:
                c.dma("sp", lambda e: e.dma_start(out=d_L, in_=Lc[:].rearrange("p t h -> p (t h)")), reads=[Lc], writes=[dbg_b], partial=True)

        with c.phase():
            cast_q = []
            for tbl, c0_ in ((pu, 0), (pv, D)):
                for r0 in range(0, NEXP, 1024):
                    cast_q.append(lambda tbl=tbl, c0_=c0_, r0=r0: c.dma(
                        "pool", lambda e: e.dma_start(out=uv[r0:r0 + 1024, c0_:c0_ + D], in_=tbl[r0:r0 + 1024, :]),
                        writes=[uv_b], partial=True))

            Vaug = c.sb([128, NT, NH, 128], BF16, name="Vaug")
            dmask = c.sb([128, NH, MASKW], BF16, name="dmask")
            c.dma("sp", lambda e: e.dma_start(out=dmask[:].rearrange("p h w -> p (h w)"), in_=c_dmask), writes=[dmask])
            while cast_q:
                cast_q.pop(0)()
            QT = [c.sb([128, T], BF16, name="QT%d" % i) for i in range(2)]
            KT = [c.sb([128, T], BF16, name="KT%d" % i) for i in range(2)]
            c.op("dve", lambda e: e.memset(QT[0][64:128, :], 0.0), writes=[QT[0]])
            c.op("dve", lambda e: e.memset(QT[1][0:64, :], 0.0), writes=[QT[1]])
            NPT = 9
            ptb = [c.sb([128, 512], BF16, name="ptb%d" % i) for i in range(NPT)]
            rz = c.sb([128, 512], F32, name="rz")
            mxs = [c.sb([64, 512], BF16, name="mxs%d" % i) for i in range(2)]
            SB_ = [PS[0], PS[1], PS[4], PS[5], PS[6], PS[7]]
            OB_ = [PS[2], PS[3]]
            dm_np = consts_np["c_dmask"].astype(np.float32).reshape(128, NH, MASKW)

            jobs = [(m, h) for m in range(2) for h in range(NH)]
            pairs = []
            ngrp = 0
            for (m, h) in jobs:
                for qb in range(NG):
                    kb0 = 0 if m == 0 else max(0, 4 * qb - 16)
                    kbl = 4 * qb + 3
                    grp = []
                    for kb in range(kb0, kbl + 1):
                        Dd = 4 * qb - kb
                        c0 = max(0, -128 * Dd)
                        c1 = 512 if m == 0 else min(512, 2176 - 128 * Dd)
                        ci0 = 128 * Dd + c0 + 384
                        if m == 1 and not np.any(dm_np[:, h, ci0:ci0 + (c1 - c0)]):
                            continue
                        grp.append(dict(m=m, h=h, qb=qb, kb=kb, Dd=Dd, c0=c0, c1=c1, ci0=ci0, g=ngrp))
                    grp[0]["first"] = True
                    grp[-1]["last"] = True
                    pairs.extend(grp)
                    ngrp += 1

            def load_job(j):
                m, h = jobs[j]
                if h == 0:
                    for i in range(NT):
                        c.dma("sp", lambda e, m=m, i=i: e.dma_start(out=Vaug[:, i, :, :].rearrange("p h d -> p (h d)"),
                                                                     in_=vaug[m, i * 128:(i + 1) * 128, :]),
                              reads=[va_b[m][i]], writes=[Vaug], partial=True)
                qt_, kt_ = QT[j % 2], KT[j % 2]
                pb = (h % 2) * 64
                qrow = m * 1024 + h * 64
                krow = m * 1024 + 512 + (h // 2) * 128
                qdeps = [qk_b[qrow // 128][g] for g in range(NG)]
                kdeps = [qk_b[krow // 128][g] for g in range(NG)]
                c.dma("sp", lambda e, qt_=qt_, qrow=qrow, pb=pb: e.dma_start(out=qt_[pb:pb + 64, :], in_=qkT[qrow:qrow + 64, :]),
                      reads=qdeps, writes=[qt_], partial=True)
                c.dma("sp", lambda e, kt_=kt_, krow=krow: e.dma_start(out=kt_[:], in_=qkT[krow:krow + 128, :]), reads=kdeps, writes=[kt_])

            def emit_S(n):
                p = pairs[n]
                j = p["m"] * NH + p["h"]
                qt_, kt_ = QT[j % 2], KT[j % 2]
                psb = SB_[n % 6]
                kb, qb, c0, c1 = p["kb"], p["qb"], p["c0"], p["c1"]
                c.op("pe", lambda e: e.matmul(psb[:, c0:c1], lhsT=kt_[:, kb * 128:(kb + 1) * 128],
                                              rhs=qt_[:, qb * 512 + c0:qb * 512 + c1], start=True, stop=True),
                     reads=[kt_, qt_], writes=[psb])

            def emit_exp(n):
                p = pairs[n]
                m, h, kb, qb, c0, c1, Dd, ci0 = p["m"], p["h"], p["kb"], p["qb"], p["c0"], p["c1"], p["Dd"], p["ci0"]
                psb = SB_[n % 6]
                pt = ptb[n % NPT]
                if m == 0:
                    c.op("act", lambda e: e.activation(out=pt[:, c0:c1], in_=psb[:, c0:c1], func=AF.Exp, scale=0.125,
                                                       bias=biasq[:, qb, kb, h:h + 1]),
                         reads=[psb, biasq], writes=[pt])
                    if Dd <= 0:
                        c.op("dve", lambda e: e.tensor_tensor(out=pt[:, c0:c0 + 128], in0=pt[:, c0:c0 + 128], in1=trib[:], op=ALU.mult),
                             reads=[pt, trib], writes=[pt])
                else:
                    c.op("act", lambda e: e.activation(out=pt[:, c0:c1], in_=psb[:, c0:c1], func=AF.Exp, scale=0.125),
                         reads=[psb], writes=[pt])
                    c.op("dve", lambda e: e.tensor_tensor(out=pt[:, c0:c1], in0=pt[:, c0:c1], in1=dmask[:, h, ci0:ci0 + (c1 - c0)], op=ALU.mult),
                         reads=[pt, dmask], writes=[pt])

            def emit_pv(n):
                p = pairs[n]
                m, h, kb, qb, c0, c1 = p["m"], p["h"], p["kb"], p["qb"], p["c0"], p["c1"]
                pt = ptb[n % NPT]
                po = OB_[p["g"] % 2]
                c.op("pe", lambda e: e.matmul(po[:, c0:c1], lhsT=Vaug[:, kb, h, :], rhs=pt[:, c0:c1],
                                              start=bool(p.get("first")), stop=bool(p.get("last")), skip_group_check=True),
                     reads=[Vaug, pt], writes=[po], partial=True)
                if p.get("last"):
                    mx = mxs[p["g"] % 2]
                    if m == 1:
                        c.op("act", lambda e: e.activation(out=rz[64:128, :], in_=po[64:128, :], func=AF.Ln), reads=[po], writes=[rz])
                        c.op("act", lambda e: e.activation(out=rz[64:128, :], in_=rz[64:128, :], func=AF.Exp, scale=-1.0), reads=[rz], writes=[rz])
                    else:
                        c.op("dve", lambda e: e.reciprocal(out=rz[64:128, :], in_=po[64:128, :]), reads=[po], writes=[rz])
                    c.op("dve", lambda e: e.tensor_tensor(out=mx[:], in0=po[0:64, :], in1=rz[64:128, :], op=ALU.mult),
                         reads=[po, rz], writes=[mx])
                    mrow = m * 512 + h * 64
                    c.dma("sp", lambda e: e.dma_start(out=mixT[mrow:mrow + 64, qb * 512:(qb + 1) * 512], in_=mx[:]),
                          reads=[mx], writes=[mx_b[m * 8 + h][qb]])

            BS = 3
            for mm in range(2):
                idxs = [n for n in range(len(pairs)) if pairs[n]["m"] == mm]
                batches = [idxs[i_:i_ + BS] for i_ in range(0, len(idxs), BS)]
                NB = len(batches)
                loaded = set()
                for k in range(NB + 2):
                    if k < NB:
                        for n in batches[k]:
                            j = pairs[n]["m"] * NH + pairs[n]["h"]
                            if j not in loaded:
                                load_job(j)
                                loaded.add(j)
                                if (j + 1) < (mm + 1) * NH and (j + 1) not in loaded:
                                    load_job(j + 1)
                                    loaded.add(j + 1)
                            emit_S(n)
                    if 1 <= k <= NB:
                        for n in batches[k - 1]:
                            emit_exp(n)
                    if 2 <= k <= NB + 1:
                        for n in batches[k - 2]:
                            emit_pv(n)

            while cast_q:
                cast_q.pop(0)()

        with c.phase():
            woutb = c.sb([128, 8, D], BF16, name="woutb")
            Ws = c.sb([128, 8, 2048], BF16, name="Ws")
            with c.phase():
                wstage = [c.sb([128, 2048], F32, name="wst4_%d" % i) for i in range(2)]
                for cc in range(8):
                    st = wstage[cc % 2]
                    c.dma("sp", lambda e, st=st, cc=cc: e.dma_start(out=st[:, 0:D], in_=w_out[cc * 128:(cc + 1) * 128, :]), writes=[st])
                    c.op("dve", lambda e, st=st, cc=cc: e.tensor_copy(out=woutb[:, cc, :], in_=st[:, 0:D]), reads=[st], writes=[woutb], partial=True)
                wqT = c.sb([128, 16, D], BF16, name="wqT")
                skT = c.sb([128, 16, 128], BF16, name="skT")
                nb_ = 0
                for cc in range(8):
                    st = wstage[cc % 2]
                    c.dma("sp", lambda e, st=st, cc=cc: e.dma_start(out=st[:, 0:2048], in_=wq[cc * 128:(cc + 1) * 128, :]), writes=[st])
                    for q4 in range(4):
                        pb = PS[nb_ % 2]
                        nb_ += 1
                        for r in range(4):
                            hp = q4 * 4 + r
                            c.op("pe", lambda e, pb=pb, r=r, hp=hp, st=st: e.transpose(out=pb[:, r * 128:(r + 1) * 128],
                                                                                     in_=st[:, hp * 128:(hp + 1) * 128], identity=identf[:]),
                                 reads=[st, identf], writes=[pb], partial=True)
                        c.op("act", lambda e, pb=pb, q4=q4, cc=cc: e.copy(out=wqT[:, q4 * 4:(q4 + 1) * 4, cc * 128:(cc + 1) * 128],
                                                                          in_=pb[:].rearrange("p (r d) -> p r d", r=4)),
                             reads=[pb], writes=[wqT], partial=True)
                skst = c.sb([128, 16, 128], F32, name="skst")
                c.dma("sp", lambda e: e.dma_start(out=skst[:], in_=subk.rearrange("g k j -> k g j")), writes=[skst])
                for q4 in range(4):
                    pb = PS[nb_ % 2]
                    nb_ += 1
                    for r in range(4):
                        hp = q4 * 4 + r
                        c.op("pe", lambda e, pb=pb, r=r, hp=hp: e.transpose(out=pb[:, r * 128:(r + 1) * 128], in_=skst[:, hp, :], identity=identf[:]),
                             reads=[skst, identf], writes=[pb], partial=True)
                    c.op("act", lambda e, pb=pb, q4=q4: e.copy(out=skT[:, q4 * 4:(q4 + 1) * 4, :], in_=pb[:].rearrange("p (r k) -> p r k", r=4)),
                         reads=[pb], writes=[skT], partial=True)
                for cc in range(8):
                    for q4 in range(4):
                        pb = PS[nb_ % 2]
                        nb_ += 1
                        for r in range(4):
                            hp = q4 * 4 + r
                            c.op("pe", lambda e, pb=pb, r=r, hp=hp, cc=cc: e.matmul(pb[:, r * 128:(r + 1) * 128], lhsT=wqT[:, hp, cc * 128:(cc + 1) * 128],
                                                                                    rhs=skT[:, hp, :], start=True, stop=True),
                                 reads=[wqT, skT], writes=[pb], partial=True)
                        c.op("dve", lambda e, pb=pb, q4=q4, cc=cc: e.tensor_scalar(out=Ws[:, cc, q4 * 512:(q4 + 1) * 512], in0=pb[:],
                                                                                   scalar1=g2t[:, cc:cc + 1], scalar2=None, op0=ALU.mult),
                             reads=[pb, g2t], writes=[Ws], partial=True)

            NR = 12
            mts = [c.sb([128, 8, 128], BF16, name="mt%d" % i) for i in range(2)]
            hts = [c.sb([128, D], F32, name="ht%d" % i) for i in range(2)]
            xn2s = [c.sb([128, D], BF16, name="xn2_%d" % i) for i in range(2)]
            xn2T = c.sb([128, 8, 128], BF16, name="xn2T")
            sc = c.sb([128, 2048], F32, name="sc")
            top = c.sb([128, 16, 16], F32, name="top")
            idx = c.sb([128, 16, 16], U32, name="idx")
            idxf = c.sb([128, 16, 16], F32, name="idxf")
            cand = c.sb([128, NH, 256], F32, name="cand")
            ctop = c.sb([128, NH, 16], F32, name="ctop")
            cpos = c.sb([128, NH, 16], U32, name="cpos")
            ci_ = c.sb([128, 128], U32, name="ci_")
            cj_ = c.sb([128, 128], U32, name="cj_")
            cif = c.sb([128, 128], F32, name="cif")
            cjf = c.sb([128, 128], F32, name="cjf")
            oh = c.sb([128, 128, 16], F32, name="oh")
            i1s = c.sb([128, 128], F32, name="i1s")
            i2s = c.sb([128, 128], F32, name="i2s")
            eidf = c.sb([128, 128], F32, name="eidf")
            eids = [c.sb([128, 128], I32, name="eid%d" % i) for i in range(2)]
            gates = [c.sb([128, NH, 16], F32, name="gate%d" % i) for i in range(2)]
            zs = c.sb([128, NH], F32, name="zs")
            hid = c.sb([128, 128], F32, name="hid")
            gel = c.sb([128, 128], F32, name="gel")
            acol = c.sb([128, 128], F32, name="acol")
            uvgs = [c.sb([128, 2 * D], BF16, name="uvg%d" % i) for i in range(NR)]
            dgs = [c.sb([128, 128], BF16, name="dg%d" % i) for i in range(4)]
            junkbs = [c.sb([128, D], BF16, name="junkb%d" % i) for i in range(4)]
            h2 = c.sb([128, D], F32, name="h2")
            yo = [c.sb([128, D], F32, name="yo%d" % i) for i in range(2)]
            ssq2 = c.sb([128, 1], F32)
            rstd2 = c.sb([128, 1], F32)
            ssq3 = c.sb([128, 1], F32)
            rstd3 = c.sb([128, 1], F32)

            hid_c = [Buf(hid.t, "hid_c%d" % i_) for i_ in range(128)]
            gel_c = [Buf(gel.t, "gel_c%d" % i_) for i_ in range(128)]
            acol_c = [Buf(acol.t, "acol_c%d" % i_) for i_ in range(128)]
            sc_g = [Buf(sc.t, "sc_g%d" % i_) for i_ in range(16)]
            top_g = [Buf(top.t, "top_g%d" % i_) for i_ in range(16)]
            idx_g = [Buf(idx.t, "idx_g%d" % i_) for i_ in range(16)]
            cand_g = [Buf(cand.t, "cand_g%d" % i_) for i_ in range(NH)]
            ctop_g = [Buf(ctop.t, "ctop_g%d" % i_) for i_ in range(NH)]
            cpos_g = [Buf(cpos.t, "cpos_g%d" % i_) for i_ in range(NH)]

            def stageA(i):
                q = []

                def o(*a, **k):
                    q.append(lambda: c.op(*a, **k))

                def dm(*a, **k):
                    q.append(lambda: c.dma(*a, **k))

                def top16_multi(specs):
                    for (src_ap, val_ap, idx_ap, src_b, val_b, idx_b) in specs:
                        o("dve", lambda e, src_ap=src_ap, val_ap=val_ap: e.max(out=val_ap[:, 0:8], in_=src_ap), reads=[src_b], writes=[val_b])
                    for (src_ap, val_ap, idx_ap, src_b, val_b, idx_b) in specs:
                        o("dve", lambda e, src_ap=src_ap, val_ap=val_ap, idx_ap=idx_ap: e.max_index(out=idx_ap[:, 0:8], in_max=val_ap[:, 0:8], in_values=src_ap),
                          reads=[src_b, val_b], writes=[idx_b])
                    for (src_ap, val_ap, idx_ap, src_b, val_b, idx_b) in specs:
                        o("dve", lambda e, src_ap=src_ap, val_ap=val_ap: e.match_replace(out=src_ap, in_to_replace=val_ap[:, 0:8], in_values=src_ap, imm_value=-1e30),
                          reads=[val_b], writes=[src_b])
                    for (src_ap, val_ap, idx_ap, src_b, val_b, idx_b) in specs:
                        o("dve", lambda e, src_ap=src_ap, val_ap=val_ap: e.max(out=val_ap[:, 8:16], in_=src_ap), reads=[src_b], writes=[val_b], partial=True)
                    for (src_ap, val_ap, idx_ap, src_b, val_b, idx_b) in specs:
                        o("dve", lambda e, src_ap=src_ap, val_ap=val_ap, idx_ap=idx_ap: e.max_index(out=idx_ap[:, 8:16], in_max=val_ap[:, 8:16], in_values=src_ap),
                          reads=[src_b, val_b], writes=[idx_b], partial=True)

                g = i // 4
                par = i % 2
                mt, xt, ht, xs, xn2, eid, gate = mts[par], xts[par], hts[par], xss[par], xn2s[par], eids[par], gates[par]
                mdeps = [mx_b[hh][g] for hh in range(16)]
                dm("sp", lambda e: e.dma_start(out=mt[:], in_=mixT.rearrange("(c p) t -> p c t", p=128)[:, :, i * 128:(i + 1) * 128]),
                   reads=mdeps, writes=[mt])
                dm("sp", lambda e: e.dma_start(out=xt[:], in_=x[i * 128:(i + 1) * 128, :]), writes=[xt])
                for hf in range(2):
                    ph = PS[hf]
                    for k in range(8):
                        o("pe", lambda e, ph=ph, k=k, hf=hf: e.matmul(ph[:], lhsT=mt[:, k, :], rhs=woutb[:, k, hf * 512:(hf + 1) * 512],
                                                                        start=(k == 0), stop=(k == 7)),
                          reads=[mt, woutb], writes=[ph], partial=True)
                    o("dve", lambda e, ph=ph, hf=hf: e.tensor_tensor(out=ht[:, hf * 512:(hf + 1) * 512], in0=ph[:],
                                                                     in1=xt[:, hf * 512:(hf + 1) * 512], op=ALU.add),
                      reads=[ph, xt], writes=[ht], partial=True)
                o("act", lambda e: e.activation(out=junk[:], in_=ht[:], func=AF.Square, accum_out=ssq2[:]), reads=[ht], writes=[junk, ssq2])
                o("act", lambda e: e.activation(out=rstd2[:], in_=ssq2[:], func=AF.Sqrt, scale=1.0 / D, bias=eps[:]),
                  reads=[ssq2, eps], writes=[rstd2])
                o("dve", lambda e: e.reciprocal(out=rstd2[:], in_=rstd2[:]), reads=[rstd2], writes=[rstd2])
                o("act", lambda e: e.activation(out=xs[:], in_=ht[:], func=AF.Copy, scale=rstd2[:]), reads=[ht, rstd2], writes=[xs])
                o("dve", lambda e: e.scalar_tensor_tensor(out=xn2[:], in0=ht[:], scalar=rstd2[:], in1=g2bc[:], op0=ALU.mult, op1=ALU.mult),
                  reads=[ht, rstd2, g2bc], writes=[xn2])
                pT = PS[0]
                pTv = pT.t[:].bitcast(BF16)
                for k in range(8):
                    o("pe", lambda e, k=k: e.transpose(out=pTv[:, k * 128:(k + 1) * 128], in_=xs[:, k * 128:(k + 1) * 128], identity=identb[:]),
                      reads=[xs, identb], writes=[pT], partial=True)
                o("dve", lambda e: e.tensor_copy(out=xn2T[:], in_=pTv.rearrange("p (k t) -> p k t", k=8)), reads=[pT], writes=[xn2T])
                for q4 in range(4):
                    pb = PS[4 + (q4 % 2)]
                    for k in range(8):
                        o("pe", lambda e, pb=pb, k=k, q4=q4: e.matmul(pb[:], lhsT=xn2T[:, k, :], rhs=Ws[:, k, q4 * 512:(q4 + 1) * 512],
                                                                      start=(k == 0), stop=(k == 7)),
                          reads=[xn2T, Ws], writes=[pb], partial=True)
                    o("act", lambda e, pb=pb, q4=q4: e.copy(out=sc[:, q4 * 512:(q4 + 1) * 512], in_=pb[:]), reads=[pb], writes=sc_g[4 * q4:4 * q4 + 4])
                top16_multi([(sc[:, gq * 128:(gq + 1) * 128], top[:, gq, :], idx[:, gq, :], sc_g[gq], top_g[gq], idx_g[gq]) for gq in range(16)])
                o("dve", lambda e: e.tensor_copy(out=idxf[:], in_=idx[:]), reads=idx_g, writes=[idxf])
                top4 = top[:].rearrange("p (h two) k -> p h two k", two=2)
                o("dve", lambda e: e.tensor_tensor(
                    out=cand[:].rearrange("p h (a b) -> p h a b", a=16),
                    in0=top4[:, :, 0, :].unsqueeze(3).to_broadcast([128, NH, 16, 16]),
                    in1=top4[:, :, 1, :].unsqueeze(2).to_broadcast([128, NH, 16, 16]), op=ALU.add),
                    reads=top_g, writes=cand_g)
                top16_multi([(cand[:, hh, :], ctop[:, hh, :], cpos[:, hh, :], cand_g[hh], ctop_g[hh], cpos_g[hh]) for hh in range(NH)])
                o("dve", lambda e: e.tensor_tensor(out=gate[:], in0=ctop[:], in1=ctop[:, :, 0:1].to_broadcast([128, NH, 16]), op=ALU.subtract),
                  reads=ctop_g, writes=[gate])
                o("act", lambda e: e.activation(out=gate[:], in_=gate[:], func=AF.Exp), reads=[gate], writes=[gate])
                o("dve", lambda e: e.tensor_reduce(out=zs[:], in_=gate[:], op=ALU.add, axis=AX.X), reads=[gate], writes=[zs])
                o("dve", lambda e: e.reciprocal(out=zs[:], in_=zs[:]), reads=[zs], writes=[zs])
                o("dve", lambda e: e.tensor_tensor(out=gate[:], in0=gate[:], in1=zs[:].unsqueeze(2).to_broadcast([128, NH, 16]), op=ALU.mult),
                  reads=[gate, zs], writes=[gate])
                cp2 = cpos[:].rearrange("p h k -> p (h k)")
                o("dve", lambda e: e.tensor_single_scalar(out=ci_[:], in_=cp2, scalar=4, op=ALU.logical_shift_right), reads=cpos_g, writes=[ci_])
                o("dve", lambda e: e.tensor_single_scalar(out=cj_[:], in_=cp2, scalar=15, op=ALU.bitwise_and), reads=cpos_g, writes=[cj_])
                o("dve", lambda e: e.tensor_copy(out=cif[:], in_=ci_[:]), reads=[ci_], writes=[cif])
                o("dve", lambda e: e.tensor_copy(out=cjf[:], in_=cj_[:]), reads=[cj_], writes=[cjf])
                idx4 = idxf[:].rearrange("p (h two) k -> p h two k", two=2)
                for (srcf, pp, dst) in ((cif, 0, i1s), (cjf, 1, i2s)):
                    o("dve", lambda e, srcf=srcf: e.tensor_tensor(
                        out=oh[:], in0=srcf[:].unsqueeze(2).to_broadcast([128, 128, 16]),
                        in1=iota16[:].unsqueeze(1).to_broadcast([128, 128, 16]), op=ALU.is_equal),
                        reads=[srcf, iota16], writes=[oh])
                    o("dve", lambda e, pp=pp: e.tensor_tensor(
                        out=oh[:].rearrange("p (h k) i -> p h k i", h=NH), in0=oh[:].rearrange("p (h k) i -> p h k i", h=NH),
                        in1=idx4[:, :, pp, :].unsqueeze(2).to_broadcast([128, NH, 16, 16]), op=ALU.mult),
                        reads=[oh, idxf], writes=[oh])
                    o("dve", lambda e, dst=dst: e.tensor_reduce(out=dst[:], in_=oh[:], op=ALU.add, axis=AX.X), reads=[oh], writes=[dst])
                o("dve", lambda e: e.scalar_tensor_tensor(out=eidf[:], in0=i1s[:], scalar=128.0, in1=i2s[:], op0=ALU.mult, op1=ALU.add),
                  reads=[i1s, i2s], writes=[eidf])
                o("dve", lambda e: e.tensor_copy(out=eid[:], in_=eidf[:]), reads=[eidf], writes=[eid])
                return q

            def stageB(i, qn, epi_prev):
                par = i % 2
                ht, xn2, eid, gate = hts[par], xn2s[par], eids[par], gates[par]
                gflat = gate[:].rearrange("p h k -> p (h k)")
                PA = [PS[2], PS[3]] if par == 0 else [PS[6], PS[7]]

                def slot_tail(s):
                    uvg = uvgs[s % NR]
                    dg = dgs[s % 4]
                    c.op("act", lambda e: e.activation(out=gel[:, s:s + 1], in_=hid[:, s:s + 1], func=AF.Gelu), reads=[hid_c[s]], writes=[gel_c[s]])
                    c.op("act", lambda e: e.activation(out=acol[:, s:s + 1], in_=gel[:, s:s + 1], func=AF.Copy, scale=gflat[:, s:s + 1]),
                         reads=[gel_c[s], gate], writes=[acol_c[s]])
                    c.op("act", lambda e: e.activation(out=dg[:], in_=identb[:], func=AF.Copy, scale=acol[:, s:s + 1]),
                         reads=[identb, acol_c[s]], writes=[dg])
                    for hf in range(2):
                        c.op("pe", lambda e, hf=hf: e.matmul(PA[hf][:], lhsT=dg[:], rhs=uvg[:, D + hf * 512:D + (hf + 1) * 512],
                                                             start=(s == 0), stop=(s == 127)),
                             reads=[dg, uvg], writes=[PA[hf]], partial=True)

                LAG = 2
                per = (len(qn) + 119) // 120 if qn else 0
                for s in range(128):
                    uvg = uvgs[s % NR]
                    c.dma("pool", lambda e, uvg=uvg, s=s: e.indirect_dma_start(
                        out=uvg[:], out_offset=None, in_=uv, in_offset=bass.IndirectOffsetOnAxis(ap=eid[:, s:s + 1], axis=0)),
                        reads=[eid, uv_b], writes=[uvg])
                    jb = junkbs[s % 4]
                    c.op("dve", lambda e, uvg=uvg, s=s, jb=jb: e.scalar_tensor_tensor(out=jb[:], in0=xn2[:], scalar=1.0, in1=uvg[:, 0:D],
                                                                                     op0=ALU.mult, op1=ALU.mult, accum_out=hid[:, s:s + 1]),
                         reads=[xn2, uvg], writes=[jb, hid_c[s]])
                    for _ in range(per):
                        if qn:
                            qn.pop(0)()
                    if s == 2:
                        while epi_prev:
                            epi_prev.pop(0)()
                    if s >= LAG:
                        slot_tail(s - LAG)
                for s in range(128 - LAG, 128):
                    slot_tail(s)
                while qn:
                    qn.pop(0)()
                if dbg:
                    c.dma("sp", lambda e: e.dma_start(out=d_hid[i * 128:(i + 1) * 128, :], in_=hid[:]), reads=hid_c, writes=[dbg_b], partial=True)
                    c.dma("sp", lambda e: e.dma_start(out=d_eid[i * 128:(i + 1) * 128, :], in_=eid[:]), reads=[eid], writes=[dbg_b], partial=True)
                    c.dma("sp", lambda e: e.dma_start(out=d_gate[i * 128:(i + 1) * 128, :], in_=gflat), reads=[gate], writes=[dbg_b], partial=True)
                    c.dma("sp", lambda e: e.dma_start(out=d_h[i * 128:(i + 1) * 128, :], in_=ht[:]), reads=[ht], writes=[dbg_b], partial=True)
                epi = []

                def epilogue():
                    for hf in range(2):
                        c.op("dve", lambda e, hf=hf: e.tensor_tensor(out=h2[:, hf * 512:(hf + 1) * 512], in0=PA[hf][:],
                                                                     in1=ht[:, hf * 512:(hf + 1) * 512], op=ALU.add),
                             reads=[PA[hf], ht], writes=[h2], partial=True)
                    c.op("act", lambda e: e.activation(out=junk[:], in_=h2[:], func=AF.Square, accum_out=ssq3[:]), reads=[h2], writes=[junk, ssq3])
                    c.op("act", lambda e: e.activation(out=rstd3[:], in_=ssq3[:], func=AF.Sqrt, scale=1.0 / D, bias=eps[:]),
                         reads=[ssq3, eps], writes=[rstd3])
                    c.op("dve", lambda e: e.reciprocal(out=rstd3[:], in_=rstd3[:]), reads=[rstd3], writes=[rstd3])
                    yt = yo[i % 2]
                    c.op("dve", lambda e: e.scalar_tensor_tensor(out=yt[:], in0=h2[:], scalar=rstd3[:], in1=gfbc[:], op0=ALU.mult, op1=ALU.mult),
                         reads=[h2, rstd3, gfbc], writes=[yt])
                    c.dma("sp", lambda e: e.dma_start(out=y[i * 128:(i + 1) * 128, :], in_=yt[:]), reads=[yt], writes=[y_b[i]])

                epi.append(epilogue)
                return epi

            for th in stageA(0):
                th()
            epi_prev = []
            for i in range(NT):
                qn = stageA(i + 1) if i + 1 < NT else []
                epi_prev = stageB(i, qn, epi_prev)
            while epi_prev:
                epi_prev.pop(0)()
        allb = list(y_b) + [dbg_b]
        if dbg:
            allb += [b for row in qk_b for b in row] + [b for row in va_b for b in row] + [b for row in mx_b for b in row]
        c.finish(allb, "sp")
        c.emit()
    return nc


def _in_map(xb, p, consts):
    m = {"x": np.ascontiguousarray(xb)}
    m.update(p)
    m.update(consts)
    return m


def _prep_params(norm1_g, w_in, b_forget, w_out, norm2_g, peer_wq, peer_subkeys, peer_u, peer_v, normf_g):
    f = lambda a: np.ascontiguousarray(np.asarray(a, dtype=np.float32))
    return {
        "g1T": f(np.asarray(norm1_g)[0].reshape(8, 128).T),
        "w_in": f(np.asarray(w_in)[0]),
        "b_forget": f(np.asarray(b_forget)[0].reshape(1, NH)),
        "w_out": f(np.asarray(w_out)[0]),
        "g2T": f(np.asarray(norm2_g)[0].reshape(8, 128).T),
        "g2": f(np.asarray(norm2_g)[0].reshape(1, D)),
        "peer_wq": f(np.asarray(peer_wq)[0]),
        "peer_subkeys": f(np.asarray(peer_subkeys)[0].reshape(16, 128, 128)),
        "peer_u": f(np.asarray(peer_u)[0]),
        "peer_v": f(np.asarray(peer_v)[0]),
        "normf_g": f(np.asarray(normf_g).reshape(1, D)),
    }


_NC_CACHE = {}


def kernel(x, norm1_g, w_in, b_forget, w_out, norm2_g, peer_wq, peer_subkeys, peer_u, peer_v, normf_g):
    x = np.asarray(x, dtype=np.float32)
    B, T, _ = x.shape
    p = _prep_params(norm1_g, w_in, b_forget, w_out, norm2_g, peer_wq, peer_subkeys, peer_u, peer_v, normf_g)
    consts = _consts()
    if T not in _NC_CACHE:
        _NC_CACHE[T] = build(T)
    nc = _NC_CACHE[T]
    in_maps = [_in_map(x[b], p, consts) for b in range(B)]
    res = run_bass_kernel_spmd(nc, in_maps, core_ids=list(range(B)))
    return np.stack([np.asarray(r["y"], dtype=np.float32) for r in res.results], axis=0)
```
